# Optimizing a Trainium2 kernel written in Bass

```python
import numpy as np
import jax, jax.numpy as jnp
from jax import lax

D_MODEL = 1024
BATCH = 16
SEQ = 2048
DEPTH = 2

N_A_LAYERS = DEPTH // 2
N_B_LAYERS = DEPTH - N_A_LAYERS
EPS = 1e-6
NEG_INF = -1e30
ROPE_THETA = 10000.0

GLA_HEADS = 4
GLA_DK = D_MODEL // (2 * GLA_HEADS)
GLA_DV = D_MODEL // GLA_HEADS
GLA_GATE_RANK = 16
GLA_TAU = 16.0
GLA_CHUNK = 64
GLA_IN = 2 * GLA_HEADS * GLA_DK + 2 * GLA_HEADS * GLA_DV + GLA_GATE_RANK

NSA_HEADS = 16
NSA_KV_HEADS = 4
NSA_GROUP = NSA_HEADS // NSA_KV_HEADS
NSA_HEAD_DIM = D_MODEL // NSA_HEADS
CMP_BLOCK = 32
CMP_STRIDE = 16
CMP_HIDDEN = 4 * NSA_HEAD_DIM
SEL_BLOCK = 64
SEL_TOPK = 16
WINDOW = 512
NSA_QCHUNK = 32
NSA_IN = NSA_HEADS * NSA_HEAD_DIM + 3 * NSA_HEADS
NSA_KV_OUT = 6 * NSA_KV_HEADS * NSA_HEAD_DIM
FORCE_SCORE = 1e4

FFN_DIM = 2816
CONV_WIDTH = 3

kernel_name = 'yoco_gla_nsa_convffn_trunk'


def rmsnorm(x, g):
    x32 = x.astype(jnp.float32)
    y = x32 * lax.rsqrt(jnp.mean(x32 * x32, axis=-1, keepdims=True) + EPS)
    return (y * g).astype(x.dtype)


def rope(x, pos):
    half = x.shape[-1] // 2
    inv = ROPE_THETA ** (-jnp.arange(half, dtype=jnp.float32) / half)
    ang = pos.astype(jnp.float32)[:, None] * inv[None, :]
    cos, sin = jnp.cos(ang), jnp.sin(ang)
    x1 = x[..., :half].astype(jnp.float32)
    x2 = x[..., half:].astype(jnp.float32)
    return jnp.concatenate([x1 * cos - x2 * sin, x1 * sin + x2 * cos], axis=-1).astype(x.dtype)


def masked_softmax(s, mask):
    s = jnp.where(mask, s.astype(jnp.float32), NEG_INF)
    return jnp.where(mask, jax.nn.softmax(s, axis=-1), 0.0)


def conv_ffn(h, w_up, conv_w, conv_b, w_down):
    S = h.shape[1]
    u = h @ w_up
    up = jnp.pad(u, ((0, 0), (CONV_WIDTH - 1, 0), (0, 0)))
    u = conv_b + up[:, 0:S] * conv_w[0]
    for j in range(1, CONV_WIDTH):
        u = u + up[:, j:j + S] * conv_w[j]
    gate, val = jnp.split(u, 2, axis=-1)
    return (jax.nn.silu(gate) * val) @ w_down


def gla_mixer(h, w_in, w_alpha_up, b_alpha, g_out, w_o):
    Bsz, S, _ = h.shape
    H, DK, DV, C = GLA_HEADS, GLA_DK, GLA_DV, GLA_CHUNK
    NC = S // C
    proj = h @ w_in
    splits = [H * DK, 2 * H * DK, 2 * H * DK + H * DV, 2 * H * DK + 2 * H * DV]
    q, k, v, r, a_low = jnp.split(proj, splits, axis=-1)
    log_a = jax.nn.log_sigmoid((a_low @ w_alpha_up + b_alpha).astype(jnp.float32)) / GLA_TAU

    def to_chunks(t, d):
        return t.astype(jnp.float32).reshape(Bsz, NC, C, H, d).transpose(1, 0, 3, 2, 4)

    qc = to_chunks(q, DK) * DK ** -0.5
    kc = to_chunks(k, DK)
    vc = to_chunks(v, DV)
    gc = to_chunks(log_a, DK)
    causal = jnp.tril(jnp.ones((C, C), dtype=bool))[:, :, None]

    def step(state, inp):
        q_, k_, v_, g_ = inp
        b = jnp.cumsum(g_, axis=2)
        o_inter = jnp.einsum('bhtk,bhkv->bhtv', q_ * jnp.exp(b), state)
        diff = b[:, :, :, None, :] - b[:, :, None, :, :]
        decay = jnp.where(causal, jnp.exp(jnp.minimum(diff, 0.0)), 0.0)
        att = jnp.einsum('bhtk,bhsk,bhtsk->bhts', q_, k_, decay)
        o_intra = jnp.einsum('bhts,bhsv->bhtv', att, v_)
        b_last = b[:, :, -1:, :]
        new_state = state * jnp.exp(b_last[:, :, 0, :, None]) + jnp.einsum(
            'bhsk,bhsv->bhkv', k_ * jnp.exp(b_last - b), v_)
        return new_state, o_inter + o_intra

    state0 = jnp.zeros((Bsz, H, DK, DV), jnp.float32)
    _, o = lax.scan(step, state0, (qc, kc, vc, gc))
    o = o.transpose(1, 0, 3, 2, 4).reshape(Bsz, S, H, DV)
    o = o * lax.rsqrt(jnp.mean(o * o, axis=-1, keepdims=True) + EPS) * g_out
    o = o.reshape(Bsz, S, H * DV).astype(h.dtype) * jax.nn.silu(r)
    return o @ w_o


def compress_blocks(t_raw, pe, w1, w2):
    Bsz, Hk, S, Dh = t_raw.shape
    n_cmp = (S - CMP_BLOCK) // CMP_STRIDE + 1
    idx = np.arange(n_cmp)[:, None] * CMP_STRIDE + np.arange(CMP_BLOCK)[None, :]
    blocks = (t_raw[:, :, idx, :] + pe).reshape(Bsz, Hk, n_cmp, CMP_BLOCK * Dh)
    return jax.nn.gelu(blocks @ w1) @ w2


def cmp_to_sel_weights(S):
    n_cmp = (S - CMP_BLOCK) // CMP_STRIDE + 1
    n_sel = S // SEL_BLOCK
    c0 = np.arange(n_cmp)[:, None] * CMP_STRIDE
    s0 = np.arange(n_sel)[None, :] * SEL_BLOCK
    ov = np.clip(np.minimum(c0 + CMP_BLOCK, s0 + SEL_BLOCK) - np.maximum(c0, s0), 0, None)
    return (ov / CMP_BLOCK).astype(np.float32)


def nsa_shared_kv(x, g_kv, w_kv, pe_k, pe_v, wk1, wk2, wv1, wv2):
    Bsz, S, _ = x.shape
    hn = rmsnorm(x, g_kv)
    kv = (hn @ w_kv).reshape(Bsz, S, 6, NSA_KV_HEADS, NSA_HEAD_DIM).transpose(2, 0, 3, 1, 4)
    pos = jnp.arange(S)
    k_cmp = compress_blocks(kv[0], pe_k, wk1, wk2)
    v_cmp = compress_blocks(kv[1], pe_v, wv1, wv2)
    k_sel = rope(kv[2], pos)
    k_win = rope(kv[4], pos)
    return k_cmp, v_cmp, k_sel, kv[3], k_win, kv[5]


gather_blocks = jax.vmap(jax.vmap(lambda blocks, ix: blocks[ix]))


def nsa_mixer(h, w_in, w_o, k_cmp, v_cmp, k_sel, v_sel, k_win, v_win):
    Bsz, S, _ = h.shape
    Hk, G, Dh, SB, QC = NSA_KV_HEADS, NSA_GROUP, NSA_HEAD_DIM, SEL_BLOCK, NSA_QCHUNK
    n_cmp = k_cmp.shape[2]
    n_sel = S // SB
    top = min(SEL_TOPK, n_sel)
    scale = Dh ** -0.5
    pos = jnp.arange(S)
    proj = h @ w_in
    q = proj[..., :NSA_HEADS * Dh].reshape(Bsz, S, Hk, G, Dh).transpose(0, 2, 3, 1, 4)
    gates = jax.nn.sigmoid(proj[..., NSA_HEADS * Dh:].astype(jnp.float32))
    gates = gates.reshape(Bsz, S, 3, Hk, G).transpose(2, 0, 3, 4, 1)[..., None]

    cmp_end = jnp.arange(n_cmp) * CMP_STRIDE + CMP_BLOCK - 1
    p_cmp = masked_softmax(jnp.einsum('bkgsd,bknd->bkgsn', q, k_cmp) * scale,
                           cmp_end[None, :] <= pos[:, None])
    o_cmp = jnp.einsum('bkgsn,bknd->bkgsd', p_cmp, v_cmp)

    imp = jnp.einsum('bkgsn,nj->bksj', p_cmp, jnp.asarray(cmp_to_sel_weights(S)))
    blk = jnp.arange(n_sel)[None, :]
    cur = (pos // SB)[:, None]
    forced = (blk == 0) | (blk == cur) | (blk == cur - 1)
    imp = jnp.where(blk > cur, -1.0, jnp.where(forced, FORCE_SCORE, imp))
    _, sel_idx = lax.top_k(imp, top)

    q_rot = rope(q, pos)
    k_blocks = k_sel.reshape(Bsz, Hk, n_sel, SB, Dh)
    v_blocks = v_sel.reshape(Bsz, Hk, n_sel, SB, Dh)
    pad = ((0, 0), (0, 0), (WINDOW, 0), (0, 0))
    k_win_pad = jnp.pad(k_win, pad)
    v_win_pad = jnp.pad(v_win, pad)

    def chunk(c):
        t0 = c * QC
        qc = lax.dynamic_slice_in_dim(q_rot, t0, QC, axis=3)
        qpos = t0 + jnp.arange(QC)
        idx = lax.dynamic_slice_in_dim(sel_idx, t0, QC, axis=2)
        ks = gather_blocks(k_blocks, idx).reshape(Bsz, Hk, QC, top * SB, Dh)
        vs = gather_blocks(v_blocks, idx).reshape(Bsz, Hk, QC, top * SB, Dh)
        kpos = (idx[..., None] * SB + jnp.arange(SB)).reshape(Bsz, Hk, 1, QC, top * SB)
        p_sel = masked_softmax(jnp.einsum('bkgqd,bkqmd->bkgqm', qc, ks) * scale,
                               kpos <= qpos[:, None])
        o_sel = jnp.einsum('bkgqm,bkqmd->bkgqd', p_sel, vs)
        kw = lax.dynamic_slice_in_dim(k_win_pad, t0, WINDOW + QC, axis=2)
        vw = lax.dynamic_slice_in_dim(v_win_pad, t0, WINDOW + QC, axis=2)
        kwpos = t0 - WINDOW + jnp.arange(WINDOW + QC)
        dist = qpos[:, None] - kwpos[None, :]
        win_mask = (kwpos[None, :] >= 0) & (dist >= 0) & (dist < WINDOW)
        p_win = masked_softmax(jnp.einsum('bkgqd,bknd->bkgqn', qc, kw) * scale, win_mask)
        o_win = jnp.einsum('bkgqn,bknd->bkgqd', p_win, vw)
        return o_sel, o_win

    o_sel, o_win = lax.map(chunk, jnp.arange(S // QC))
    o_sel = jnp.moveaxis(o_sel, 0, 3).reshape(Bsz, Hk, G, S, Dh)
    o_win = jnp.moveaxis(o_win, 0, 3).reshape(Bsz, Hk, G, S, Dh)
    o = gates[0] * o_cmp + gates[1] * o_sel + gates[2] * o_win
    o = o.transpose(0, 3, 1, 2, 4).reshape(Bsz, S, NSA_HEADS * Dh).astype(h.dtype)
    return o @ w_o


def setup_inputs(seed: int = 0) -> dict:
    key = jax.random.key(seed)
    ks = jax.random.split(key, 32)
    f32 = jnp.float32

    def nrm(k, shape, fan_in):
        return jax.random.normal(k, shape, f32) * fan_in ** -0.5

    def gain(k, shape):
        return 1.0 + 0.02 * jax.random.normal(k, shape, f32)

    two_f = 2 * FFN_DIM
    return {
        'x': jax.random.normal(ks[0], (BATCH, SEQ, D_MODEL), f32),
        'norm_mix': gain(ks[1], (DEPTH, D_MODEL)),
        'norm_ffn': gain(ks[2], (DEPTH, D_MODEL)),
        'gla_w_in': nrm(ks[3], (N_A_LAYERS, D_MODEL, GLA_IN), D_MODEL),
        'gla_w_alpha_up': nrm(ks[4], (N_A_LAYERS, GLA_GATE_RANK, GLA_HEADS * GLA_DK), GLA_GATE_RANK),
        'gla_b_alpha': 0.1 * jax.random.normal(ks[5], (N_A_LAYERS, GLA_HEADS * GLA_DK), f32),
        'gla_norm': gain(ks[6], (N_A_LAYERS, GLA_HEADS, GLA_DV)),
        'gla_w_o': nrm(ks[7], (N_A_LAYERS, GLA_HEADS * GLA_DV, D_MODEL), GLA_HEADS * GLA_DV),
        'kv_norm': gain(ks[8], (D_MODEL,)),
        'nsa_w_kv': nrm(ks[9], (D_MODEL, NSA_KV_OUT), D_MODEL),
        'cmp_pe_k': 0.02 * jax.random.normal(ks[10], (CMP_BLOCK, NSA_HEAD_DIM), f32),
        'cmp_pe_v': 0.02 * jax.random.normal(ks[11], (CMP_BLOCK, NSA_HEAD_DIM), f32),
        'cmp_k_w1': nrm(ks[12], (CMP_BLOCK * NSA_HEAD_DIM, CMP_HIDDEN), CMP_BLOCK * NSA_HEAD_DIM),
        'cmp_k_w2': nrm(ks[13], (CMP_HIDDEN, NSA_HEAD_DIM), CMP_HIDDEN),
        'cmp_v_w1': nrm(ks[14], (CMP_BLOCK * NSA_HEAD_DIM, CMP_HIDDEN), CMP_BLOCK * NSA_HEAD_DIM),
        'cmp_v_w2': nrm(ks[15], (CMP_HIDDEN, NSA_HEAD_DIM), CMP_HIDDEN),
        'nsa_w_in': nrm(ks[16], (N_B_LAYERS, D_MODEL, NSA_IN), D_MODEL),
        'nsa_w_o': nrm(ks[17], (N_B_LAYERS, NSA_HEADS * NSA_HEAD_DIM, D_MODEL), NSA_HEADS * NSA_HEAD_DIM),
        'ffn_w_up': nrm(ks[18], (DEPTH, D_MODEL, two_f), D_MODEL),
        'ffn_conv_w': nrm(ks[19], (DEPTH, CONV_WIDTH, two_f), CONV_WIDTH),
        'ffn_conv_b': 0.02 * jax.random.normal(ks[20], (DEPTH, two_f), f32),
        'ffn_w_down': nrm(ks[21], (DEPTH, FFN_DIM, D_MODEL), FFN_DIM),
        'norm_final': gain(ks[22], (D_MODEL,)),
    }


def reference(x, norm_mix, norm_ffn, gla_w_in, gla_w_alpha_up, gla_b_alpha, gla_norm, gla_w_o,
              kv_norm, nsa_w_kv, cmp_pe_k, cmp_pe_v, cmp_k_w1, cmp_k_w2, cmp_v_w1, cmp_v_w2,
              nsa_w_in, nsa_w_o, ffn_w_up, ffn_conv_w, ffn_conv_b, ffn_w_down, norm_final):
    shared = None
    for layer in range(DEPTH):
        if layer < N_A_LAYERS:
            hn = rmsnorm(x, norm_mix[layer])
            x = x + gla_mixer(hn, gla_w_in[layer], gla_w_alpha_up[layer], gla_b_alpha[layer],
                              gla_norm[layer], gla_w_o[layer])
        else:
            if layer == N_A_LAYERS:
                shared = nsa_shared_kv(x, kv_norm, nsa_w_kv, cmp_pe_k, cmp_pe_v,
                                       cmp_k_w1, cmp_k_w2, cmp_v_w1, cmp_v_w2)
            b = layer - N_A_LAYERS
            hn = rmsnorm(x, norm_mix[layer])
            x = x + nsa_mixer(hn, nsa_w_in[b], nsa_w_o[b], *shared)
        hn = rmsnorm(x, norm_ffn[layer])
        x = x + conv_ffn(hn, ffn_w_up[layer], ffn_conv_w[layer], ffn_conv_b[layer], ffn_w_down[layer])
    return rmsnorm(x, norm_final)
```

```python
import numpy as np
import concourse.bass as bass
import concourse.mybir as mybir
from concourse.bass_utils import run_bass_kernel_spmd

F32 = mybir.dt.float32
BF16 = mybir.dt.bfloat16
AF = mybir.ActivationFunctionType
ALU = mybir.AluOpType
AX = mybir.AxisListType

SEM_CH = 16000
N_DMA_SEMS = 24


def _region(ap):
    t = ap.tensor
    cls = type(t).__name__
    if 'DRam' in cls or 'Dram' in cls or 'DRAM' in cls:
        return None
    pairs = ap.ap
    shape = t.shape
    pstride = 1
    for s in shape[1:]:
        pstride *= s
    off = int(ap.offset)
    p0 = off // pstride
    f0 = off % pstride
    if pairs[0][0] == pstride or pairs[0][1] == 1:
        pc = pairs[0][1]
        rest = pairs[1:]
    else:
        pc = 1
        rest = pairs
    ext = 0
    for st, cnt in rest:
        ext += (cnt - 1) * abs(st)
    f1 = f0 + ext + 1
    if 'PSum' in cls or 'Psum' in cls or 'PSUM' in cls:
        be = 2048 // (2 if 'bfloat16' in str(ap.dtype) or 'float16' in str(ap.dtype) else 4)
        return (t.name, 0, 128, (f0 // be) * be, ((f1 + be - 1) // be) * be, True)
    return (t.name, p0, p0 + pc, f0, f1, False)


def _overlap(a, b):
    return a[1] < b[2] and b[1] < a[2] and a[3] < b[4] and b[3] < a[4]


def _contains(a, b):
    return a[1] <= b[1] and b[2] <= a[2] and a[3] <= b[3] and b[4] <= a[4]


class Prog:
    ENGS = ('pe', 'act', 'dve', 'pool', 'sp')

    def __init__(self, nc):
        self.nc = nc
        self.ops = []
        self.hist = {}
        self.bar_pending = set()
        self.bar_deps = set()
        self.bar_deps_prev = set()
        self.bar_start = 0

    def barrier(self):
        last = {}
        dmas = set()
        for o in self.ops[self.bar_start:]:
            if o['dma']:
                dmas.add(o['idx'])
            else:
                last[o['eng']] = o['idx']
        self.bar_deps = set(last.values()) | dmas | set(self.bar_deps_prev)
        self.bar_deps_prev = set(last.values())
        self.bar_pending = set(self.ENGS)
        self.bar_start = len(self.ops)
        self.hist = {}

    def op(self, eng, fn, reads=(), writes=(), dma=False):
        idx = len(self.ops)
        deps = set()
        if eng in self.bar_pending:
            deps |= self.bar_deps
            self.bar_pending.discard(eng)
        rr = [r for r in (_region(a) for a in reads) if r is not None]
        ww = [r for r in (_region(a) for a in writes) if r is not None]
        ww = ww + [r for r in rr if r[5]]
        rr = [r for r in rr if not r[5]]
        for r in rr:
            for rec in self.hist.get(r[0], ()):
                if rec[6] and _overlap(r, rec):
                    deps.add(rec[5])
        for w in ww:
            for rec in self.hist.get(w[0], ()):
                if _overlap(w, rec):
                    deps.add(rec[5])
        deps.discard(idx)
        for w in ww:
            lst = self.hist.setdefault(w[0], [])
            lst[:] = [rec for rec in lst if not _contains(w, rec)]
            lst.append((w[0], w[1], w[2], w[3], w[4], idx, True, eng, dma))
        for r in rr:
            lst = self.hist.setdefault(r[0], [])
            if not dma:
                lst[:] = [rec for rec in lst
                          if not ((not rec[6]) and rec[7] == eng and not rec[8] and _contains(r, rec))]
            lst.append((r[0], r[1], r[2], r[3], r[4], idx, False, eng, dma))
        self.ops.append(dict(eng=eng, fn=fn, deps=deps, dma=dma, idx=idx))
        return idx

    def emit(self, stack):
        nc = self.nc
        ops = self.ops
        signal = [False] * len(ops)
        for o in ops:
            for d in o['deps']:
                od = ops[d]
                if od['eng'] == 'pe' and o['eng'] == 'pe' and not od['dma'] and not o['dma']:
                    continue
                signal[d] = True
        eng_count = {e: 0 for e in self.ENGS}
        eng_sems = {e: [] for e in self.ENGS}
        dma_sems = [stack.enter_context(nc.semaphore(f"dsem{i}")) for i in range(N_DMA_SEMS)]
        dma_tot = [0] * N_DMA_SEMS
        dma_last = [None] * N_DMA_SEMS
        n_sw = 4
        n_hw = N_DMA_SEMS - n_sw
        dma_rr = {'sw': 0, 'hw': 0}
        token = [None] * len(ops)
        extra_dep = [None] * len(ops)
        for o in ops:
            i = o['idx']
            if o['dma']:
                kind = 'sw' if o['eng'] == 'pool' else 'hw'
                k = (dma_rr[kind] % n_sw) + n_hw if kind == 'sw' else (dma_rr[kind] % n_hw)
                dma_rr[kind] += 1
                extra_dep[i] = dma_last[k]
                dma_tot[k] += 16
                token[i] = (dma_sems[k], dma_tot[k], 16)
                dma_last[k] = i
            elif signal[i]:
                e = o['eng']
                c = eng_count[e]
                ep = c // SEM_CH
                if ep >= len(eng_sems[e]):
                    eng_sems[e].append(stack.enter_context(nc.semaphore(f"s_{e}{ep}")))
                token[i] = (eng_sems[e][ep], c % SEM_CH + 1, 1)
                eng_count[e] = c + 1
        self.n_signal = dict(eng_count)
        block = stack.enter_context(nc.Block())

        def run_engine(e, eng_obj):
            waited = {}
            my_dma = set()
            for o in ops:
                if o['eng'] != e:
                    continue
                i = o['idx']
                deps = set(o['deps'])
                if extra_dep[i] is not None:
                    deps.add(extra_dep[i])
                need = {}
                for d in deps:
                    od = ops[d]
                    if od['eng'] == 'pe' and e == 'pe' and not od['dma'] and not o['dma']:
                        continue
                    sem, val, _ = token[d]
                    key = id(sem)
                    if waited.get(key, 0) >= val:
                        continue
                    if key not in need or need[key][1] < val:
                        need[key] = (sem, val)
                for key, (sem, val) in need.items():
                    eng_obj.wait_ge(sem, val)
                    waited[key] = val
                ins = o['fn'](eng_obj)
                if token[i] is not None:
                    sem, val, inc = token[i]
                    ins.then_inc(sem, inc)
                    if o['dma']:
                        my_dma.add(i)
            fin = {}
            for i in my_dma:
                sem, val, _ = token[i]
                key = id(sem)
                if key not in fin or fin[key][1] < val:
                    fin[key] = (sem, val)
            for key, (sem, val) in fin.items():
                if waited.get(key, 0) < val:
                    eng_obj.wait_ge(sem, val)

        @block.tensor
        def _(pe):
            run_engine('pe', pe)

        @block.scalar
        def _(act):
            run_engine('act', act)

        @block.vector
        def _(dve):
            run_engine('dve', dve)

        @block.gpsimd
        def _(pool):
            run_engine('pool', pool)

        @block.sync
        def _(sp):
            run_engine('sp', sp)

    def dma(self, out, in_, eng='sp'):
        return self.op(eng, lambda e: e.dma_start(out=out, in_=in_), reads=[in_], writes=[out], dma=True)

    def mm(self, out, lhsT, rhs, start=True, stop=True, skip=False):
        rd = [lhsT, rhs] + ([] if start else [out])
        if skip:
            return self.op('pe', lambda e: e.matmul(out, lhsT, rhs, start=start, stop=stop, skip_group_check=True), reads=rd, writes=[out])
        return self.op('pe', lambda e: e.matmul(out, lhsT, rhs, start=start, stop=stop), reads=rd, writes=[out])

    def transpose(self, out, in_, ident):
        return self.op('pe', lambda e: e.transpose(out, in_, ident), reads=[in_, ident], writes=[out])

    def act(self, out, in_, func, bias=None, scale=None, accum_out=None, eng='act'):
        kw = {}
        rd = [in_]
        wr = [out]
        if bias is not None:
            kw['bias'] = bias
            if not isinstance(bias, (int, float)):
                rd.append(bias)
        if scale is not None:
            kw['scale'] = scale
            if not isinstance(scale, (int, float)):
                rd.append(scale)
        if accum_out is not None:
            kw['accum_out'] = accum_out
            wr.append(accum_out)
        return self.op(eng, lambda e: e.activation(out, in_, func, **kw), reads=rd, writes=wr)

    def tt(self, out, in0, in1, op, eng='dve'):
        return self.op(eng, lambda e: e.tensor_tensor(out, in0, in1, op), reads=[in0, in1], writes=[out])

    def ts(self, out, in0, s1, s2, op0, op1=None, eng='dve', accum_out=None):
        rd = [in0] + [s for s in (s1, s2) if s is not None and not isinstance(s, (int, float))]
        wr = [out] + ([accum_out] if accum_out is not None else [])
        kw = {}
        if op1 is not None:
            kw['op1'] = op1
        if accum_out is not None:
            kw['accum_out'] = accum_out
        return self.op(eng, lambda e: e.tensor_scalar(out, in0, s1, s2, op0, **kw), reads=rd, writes=wr)

    def stt(self, out, in0, scalar, in1, op0, op1, eng='dve'):
        rd = [in0, in1] + ([] if isinstance(scalar, (int, float)) else [scalar])
        return self.op(eng, lambda e: e.scalar_tensor_tensor(out, in0, scalar, in1, op0, op1), reads=rd, writes=[out])

    def copy(self, out, in_, eng='dve'):
        if eng == 'act':
            return self.op(eng, lambda e: e.activation(out, in_, AF.Copy), reads=[in_], writes=[out])
        return self.op(eng, lambda e: e.tensor_copy(out, in_), reads=[in_], writes=[out])

    def memset(self, ap, val, eng='pool'):
        return self.op(eng, lambda e: e.memset(ap, val), writes=[ap])


S = 2048
D = 1024
NT = 4
EPS = 1e-6
FF = 2816
NPAIR = 22
FGROUPS = [list(range(i, min(i + 4, NPAIR))) for i in range(0, NPAIR, 4)]
WINS = [(0, 684, 0), (682, 684, 684), (1364, 684, 1366)]


def host_prep(inp):
    f = lambda a: np.ascontiguousarray(a, dtype=np.float32)
    o = {}
    def rows_tiled(w):
        K, N = w.shape
        return f(w.reshape(K // 128, 128, N).transpose(1, 0, 2))
    gains = np.stack([inp['norm_mix'][0], inp['norm_mix'][1], inp['norm_ffn'][0], inp['norm_ffn'][1],
                      inp['kv_norm'], inp['norm_final']], 0)
    o['gains'] = f(gains.reshape(6, 8, 128).transpose(2, 0, 1))
    for l in range(2):
        wu = inp['ffn_w_up'][l]
        pairs = []
        for j in range(NPAIR):
            pairs.append(np.concatenate([wu[:, j * 128:(j + 1) * 128], wu[:, FF + j * 128:FF + (j + 1) * 128]], 1))
        o[f'wup{l}'] = f(np.stack([rows_tiled(p) for p in pairs], 0))
        cw = inp['ffn_conv_w'][l]; cb = inp['ffn_conv_b'][l]
        par = np.stack([cw[0], cw[1], cw[2], cb], -1)
        par = par.reshape(2, NPAIR, 128, 4).transpose(2, 1, 0, 3)
        o[f'cpar{l}'] = f(par)
        o[f'wdn{l}'] = rows_tiled(inp['ffn_w_down'][l])
    wi = inp['gla_w_in'][0]
    hw = []
    for h in range(4):
        hw.append(np.concatenate([wi[:, h * 128:(h + 1) * 128], wi[:, 512 + h * 128:512 + (h + 1) * 128],
                                  wi[:, 1024 + h * 256:1024 + (h + 1) * 256], wi[:, 2048 + h * 256:2048 + (h + 1) * 256]], 1))
    o['gla_wh'] = f(np.stack([rows_tiled(w) for w in hw], 0))
    o['gla_wa'] = rows_tiled(wi[:, 3072:3088])
    o['gla_wau'] = f(inp['gla_w_alpha_up'][0])
    o['gla_ba'] = f(np.tile(inp['gla_b_alpha'][0][None, :], (128, 1)))
    o['gla_gn'] = f(np.tile(inp['gla_norm'][0].reshape(1, 1024), (128, 1)))
    o['gla_wo'] = rows_tiled(inp['gla_w_o'][0])
    wkv = inp['nsa_w_kv']; wq = inp['nsa_w_in'][0]
    sw = lambda w: np.concatenate([w[:, 32:64], w[:, 0:32]], 1)
    kvs, qs, gs = [], [], []
    for hk in range(4):
        c = lambda kind: wkv[:, kind * 256 + hk * 64: kind * 256 + (hk + 1) * 64]
        kvs.append(rows_tiled(np.concatenate([c(0), c(1), c(2), sw(c(2)), c(4), sw(c(4)), c(3), c(5)], 1)))
        ql = []
        for g in range(4):
            h = hk * 4 + g
            ql += [wq[:, h * 64:(h + 1) * 64], sw(wq[:, h * 64:(h + 1) * 64])]
        qs.append(rows_tiled(np.concatenate(ql, 1)))
        gcols = [1024 + b * 16 + hk * 4 + g for b in range(3) for g in range(4)]
        gs.append(rows_tiled(wq[:, gcols]))
    o['nsa_wkv'] = f(np.stack(kvs, 0)); o['nsa_wq'] = f(np.stack(qs, 0)); o['nsa_wg'] = f(np.stack(gs, 0))
    o['nsa_wo2'] = rows_tiled(inp['nsa_w_o'][0])
    o['cmp_w1'] = f(np.stack([inp['cmp_k_w1'].reshape(32, 64, 256).transpose(1, 0, 2),
                              inp['cmp_v_w1'].reshape(32, 64, 256).transpose(1, 0, 2)], 0))
    o['cmp_w2'] = f(np.stack([inp['cmp_k_w2'].reshape(2, 128, 64).transpose(1, 0, 2),
                              inp['cmp_v_w2'].reshape(2, 128, 64).transpose(1, 0, 2)], 0))
    o['cmp_peT'] = f(np.stack([inp['cmp_pe_k'].T, inp['cmp_pe_v'].T], 0))
    return o


def host_consts():
    c = {}
    c['ident'] = np.eye(128, dtype=np.float32)
    s = np.arange(128)
    c['ltri'] = (s[:, None] <= s[None, :]).astype(np.float32)
    c['utri'] = (s[:, None] > s[None, :]).astype(np.float32)
    pos = np.arange(S, dtype=np.float32)
    inv = (10000.0 ** (-np.arange(32, dtype=np.float32) / 32)).astype(np.float32)
    ang = pos[None, :] * inv[:, None]
    cos = np.cos(ang).astype(np.float32); sin = np.sin(ang).astype(np.float32)
    c['ropec'] = np.concatenate([cos, cos], 0)
    c['ropes'] = np.concatenate([-sin, sin], 0)
    n = np.arange(127)
    c['cmpmask'] = ((n[:, None] * 16 + 31) <= np.arange(S)[None, :]).astype(np.float32)
    c0 = n[:, None] * 16; s0 = np.arange(32)[None, :] * 64
    ov = np.clip(np.minimum(c0 + 32, s0 + 64) - np.maximum(c0, s0), 0, None)
    c['wov'] = (ov / 32).astype(np.float32)
    blk = np.arange(32)[None, :]; cur = (np.arange(S) // 64)[:, None]
    forced = (blk == 0) | (blk == cur) | (blk == cur - 1)
    A = np.where((blk > cur) | forced, 0.0, 1.0)
    B = np.where(blk > cur, -1.0, np.where(forced, 1e4, 0.0))
    c['selA'] = A.reshape(16, 128, 32).transpose(1, 0, 2).astype(np.float32).copy()
    c['selB'] = B.reshape(16, 128, 32).transpose(1, 0, 2).astype(np.float32).copy()
    c['expand'] = (np.arange(32)[:, None] == (np.arange(S) // 64)[None, :]).astype(np.float32)
    kp = np.arange(128)[:, None, None]; rel = np.arange(8)[None, :, None]; qq = np.arange(512)[None, None, :]
    dist = qq - (rel * 128 - 512 + kp)
    c['band'] = ((dist >= 0) & (dist < 512)).astype(np.float32)
    return {k: np.ascontiguousarray(v, dtype=np.float32) for k, v in c.items()}


class K:
    def __init__(self, nc, P, stack, nseq, shapes, stop=None):
        self.nc, self.P, self.stack, self.nseq, self.stop = nc, P, stack, nseq, stop
        self.din = {}
        for name, shp in shapes.items():
            self.din[name] = nc.dram_tensor(name, list(shp), F32, kind="ExternalInput").ap()
        self.out = nc.dram_tensor("out", [nseq, S, D], F32, kind="ExternalOutput").ap()
        self.scopes = []

    def sb(self, name, shape, dt, scoped=True):
        st = self.scopes[-1] if (scoped and self.scopes) else self.stack
        self.uid = getattr(self, 'uid', 0) + 1
        return st.enter_context(self.nc.sbuf_tensor(f"sb{self.uid}_{name}", shape, dt))

    def ps(self, name, shape, dt):
        return self.stack.enter_context(self.nc.psum_tensor("ps_" + name, shape, dt))

    def push(self):
        from contextlib import ExitStack
        es = ExitStack()
        self.scopes.append(es)

    def pop(self):
        self.P.barrier()
        self.scopes.pop().close()

    def setup(self):
        P, d = self.P, self.din
        self.xT = self.sb("xT", [128, 8, S], F32, scoped=False)
        self.rstd = self.sb("rstd", [128, S], F32, scoped=False)
        self.identf = self.sb("identf", [128, 128], F32, scoped=False)
        self.identb = self.sb("identb", [128, 128], BF16, scoped=False)
        self.onesb = self.sb("onesb", [128, 128], BF16, scoped=False)
        self.gains = self.sb("gains", [128, 6, 8], F32, scoped=False)
        self.epsc = self.sb("epsc", [128, 1], F32, scoped=False)
        self.hntmp = self.sb("hntmp", [128, 1, 512], F32, scoped=False)
        self.big0 = self.ps("big0", [128, 1024], F32)
        self.big1 = self.ps("big1", [128, 1024], F32)
        self.s0 = self.ps("s0", [128, 512], F32)
        self.s1 = self.ps("s1", [128, 512], F32)
        self.s2 = self.ps("s2", [128, 512], F32)
        self.pst = self.ps("pst", [128, 1024], BF16)
        P.dma(self.identf[:], d['ident'])
        P.dma(self.identb[:], d['ident'], eng='pool')
        P.dma(self.gains[:], d['gains'])
        P.memset(self.onesb[:], 1.0)
        P.memset(self.epsc[:], EPS)

    def load_x(self, si):
        P = self.P
        self.push()
        xin = [self.sb(f"xin{i}", [128, D], F32) for i in range(2)]
        banks = [self.s0, self.s1]
        for i in range(16):
            xb = xin[i % 2]
            P.dma(xb[:], self.din['x'][si, i * 128:(i + 1) * 128, :])
            for half in range(2):
                pb = banks[half]
                for cc in range(4):
                    c = half * 4 + cc
                    P.transpose(pb[:, cc * 128:(cc + 1) * 128], xb[:, c * 128:(c + 1) * 128], self.identf[:])
                P.copy(self.xT[:, half * 4:(half + 1) * 4, i * 128:(i + 1) * 128],
                       pb[:].rearrange("p (c t) -> p c t", c=4), eng='dve' if half == 0 else 'act')
        self.pop()

    def compute_rstd(self):
        P = self.P
        self.push()
        sqb = self.sb("sqb", [128, 8, 512], BF16)
        banks = [self.s0, self.s1]
        for t in range(NT):
            ts_ = slice(t * 512, (t + 1) * 512)
            P.act(sqb[:], self.xT[:, :, ts_], AF.Square)
            pb = banks[t % 2]
            for c in range(8):
                P.mm(pb[:], self.onesb[:], sqb[:, c, :], start=(c == 0), stop=(c == 7))
            P.act(self.rstd[:, ts_], pb[:], AF.Ln, bias=self.epsc[:, 0:1], scale=1.0 / D)
            P.act(self.rstd[:, ts_], self.rstd[:, ts_], AF.Exp, scale=-0.5)
        self.pop()

    def hn_tile(self, dst, gidx, t0, n=512, flip=0):
        P = self.P
        for c in range(8):
            if c % 2 == 0:
                P.stt(dst[:, c, 0:n], self.xT[:, c, t0:t0 + n], self.gains[:, gidx, c:c + 1], self.rstd[:, t0:t0 + n],
                      ALU.mult, ALU.mult)
            else:
                tmp = self.hntmp[:, 0, 0:n]
                P.act(tmp, self.xT[:, c, t0:t0 + n], AF.Copy, scale=self.gains[:, gidx, c:c + 1])
                P.tt(dst[:, c, 0:n], tmp, self.rstd[:, t0:t0 + n], ALU.mult, eng='pool')

    def ffn(self, l):
        P, d = self.P, self.din
        self.compute_rstd()
        self.push()
        hnT = self.sb("f_hnT", [128, 8, S], BF16)
        actT = self.sb("f_actT", [128, 4, S], BF16)
        actT2 = self.sb("f_actT2", [128, 4, S], BF16)
        wu = [self.sb(f"f_wu{i}", [128, 8, 256], BF16) for i in range(3)]
        wd = [self.sb(f"f_wd{i}", [128, 4, 1024], BF16) for i in range(2)]
        cpar = self.sb("f_cpar", [128, NPAIR, 2, 4], F32)
        tg = [self.sb(f"f_tg{i}", [128, 684], F32) for i in range(2)]
        tv = [self.sb(f"f_tv{i}", [128, 684], F32) for i in range(2)]
        P.dma(cpar[:], d[f'cpar{l}'])
        for t in range(NT):
            self.hn_tile(hnT[:, :, t * 512:(t + 1) * 512], 2 + l, t * 512)
        dbanks = [self.s0, self.s1, self.s2]
        dcount = [0]
        itc = [0]
        actTs = [actT, actT2]

        def up(gi):
            grp = FGROUPS[gi]
            aT = actTs[gi % 2]
            for jj, j in enumerate(grp):
                wub = wu[j % 3]
                if j == 0:
                    P.dma(wu[0][:], d[f'wup{l}'][0], eng='pool')
                    P.dma(wu[1][:], d[f'wup{l}'][1], eng='pool')
                if j + 2 < NPAIR:
                    P.dma(wu[(j + 2) % 3][:], d[f'wup{l}'][j + 2], eng='pool')
                for (s0_, ncol, olo) in WINS:
                    tgb, tvb = tg[itc[0] % 2], tv[itc[0] % 2]
                    itc[0] += 1
                    for (pb, tb, half, silu) in ((self.big0, tgb, 0, True), (self.big1, tvb, 1, False)):
                        for (c0, cn) in ((0, 512), (512, ncol - 512)):
                            for c in range(8):
                                P.mm(pb[:, c0:c0 + cn], wub[:, c, half * 128:(half + 1) * 128],
                                     hnT[:, c, s0_ + c0:s0_ + c0 + cn], start=(c == 0), stop=(c == 7))
                            yield
                        w0 = cpar[:, j, half, 0:1]; w1 = cpar[:, j, half, 1:2]; w2 = cpar[:, j, half, 2:3]; bb = cpar[:, j, half, 3:4]
                        P.act(tb[:, 0:ncol], pb[:, 0:ncol], AF.Identity, bias=bb, scale=w2)
                        P.stt(tb[:, 1:ncol], pb[:, 0:ncol - 1], w1, tb[:, 1:ncol], ALU.mult, ALU.add)
                        P.stt(tb[:, 2:ncol], pb[:, 0:ncol - 2], w0, tb[:, 2:ncol], ALU.mult, ALU.add)
                        if silu:
                            P.act(tb[:, 0:ncol], tb[:, 0:ncol], AF.Silu)
                    lo = olo - s0_
                    P.tt(aT[:, jj, olo:s0_ + ncol], tgb[:, lo:ncol], tvb[:, lo:ncol], ALU.mult, eng='pool')
                    yield

        def down(gi):
            grp = FGROUPS[gi]
            aT = actTs[gi % 2]
            wdb = wd[gi % 2]
            if gi == 0:
                P.dma(wdb[:, 0:len(grp), :], d[f'wdn{l}'][:, grp[0]:grp[0] + len(grp), :], eng='pool')
            if gi + 1 < len(FGROUPS):
                g2 = FGROUPS[gi + 1]
                P.dma(wd[(gi + 1) % 2][:, 0:len(g2), :], d[f'wdn{l}'][:, g2[0]:g2[0] + len(g2), :], eng='pool')
            for m in range(8):
                for t in range(NT):
                    pb = dbanks[dcount[0] % 3]
                    dcount[0] += 1
                    for jj in range(len(grp)):
                        P.mm(pb[:], wdb[:, jj, m * 128:(m + 1) * 128], aT[:, jj, t * 512:(t + 1) * 512],
                             start=(jj == 0), stop=(jj == len(grp) - 1))
                    xs = self.xT[:, m, t * 512:(t + 1) * 512]
                    P.tt(xs, pb[:], xs, ALU.add)
                    yield

        def interleave(gens):
            gens = [g_ for g_ in gens if g_ is not None]
            while gens:
                for g_ in list(gens):
                    try:
                        next(g_)
                    except StopIteration:
                        gens.remove(g_)

        ng = len(FGROUPS)
        interleave([up(0)])
        for gi in range(ng):
            interleave([down(gi), up(gi + 1) if gi + 1 < ng else None])
        self.pop()

    def final_store(self, si):
        P = self.P
        self.compute_rstd()
        self.push()
        yb = [self.sb(f"o_y{i}", [128, 8, 128], F32) for i in range(2)]
        ob = [self.sb(f"o_o{i}", [128, D], F32) for i in range(2)]
        pbs = [self.big0, self.big1]
        for i in range(16):
            y = yb[i % 2]; o = ob[i % 2]; pb = pbs[i % 2]
            for c in range(8):
                P.stt(y[:, c, :], self.xT[:, c, i * 128:(i + 1) * 128], self.gains[:, 5, c:c + 1],
                      self.rstd[:, i * 128:(i + 1) * 128], ALU.mult, ALU.mult)
            for c in range(8):
                P.transpose(pb[:, c * 128:(c + 1) * 128], y[:, c, :], self.identf[:])
            P.copy(o[:, 0:512], pb[:, 0:512], eng='act')
            P.copy(o[:, 512:1024], pb[:, 512:1024], eng='dve')
            P.dma(self.out[si, i * 128:(i + 1) * 128, :], o[:])
        self.pop()

def _run_seq(self, si):
    self.load_x(si)
    if self.stop == 'load':
        return self.final_store(si)
    self.ffn(0)
    self.final_store(si)
K.run_seq = _run_seq


def _gla(self):
    P, d = self.P, self.din
    self.push()
    sb = self.sb
    oT = sb("g_oT", [128, 8, S], BF16)
    self.compute_rstd()
    self.push()
    hnT = sb("g_hnT", [128, 8, S], BF16)
    wh = [sb(f"g_wh{i}", [128, 8, 768], BF16) for i in range(2)]
    wa = sb("g_wa", [128, 8, 16], BF16)
    wau = sb("g_wau", [16, 512], F32)
    ba = sb("g_ba", [128, 512], F32)
    gn = sb("g_gn", [128, 1024], F32)
    alT = sb("g_alT", [16, S], F32)
    ltri = sb("g_ltri", [128, 128], F32)
    utri = sb("g_utri", [128, 128], F32)
    onef = sb("g_onef", [128, 128], F32)
    Sf = sb("g_Sf", [128, 256], F32)
    Sb = sb("g_Sb", [128, 256], BF16)
    NB = 2
    qk = [sb(f"g_qk{i}", [128, 256], BF16) for i in range(NB)]
    kh = [sb(f"g_kh{i}", [128, 128], BF16) for i in range(NB)]
    vv = [sb(f"g_vv{i}", [128, 256], BF16) for i in range(NB)]
    sr = [sb(f"g_sr{i}", [128, 256], F32) for i in range(NB)]
    qkT = [sb(f"g_qkT{i}", [128, 256], BF16) for i in range(NB)]
    attm = [sb(f"g_attm{i}", [128, 128], BF16) for i in range(NB)]
    ss = [sb(f"g_ss{i}", [128, 1], F32) for i in range(NB)]
    og = [sb(f"g_og{i}", [128, 256], F32) for i in range(NB)]
    sq = og
    P.dma(wa[:], d['gla_wa'], eng='pool')
    P.dma(wau[:], d['gla_wau'])
    P.dma(ba[:], d['gla_ba'])
    P.dma(gn[:], d['gla_gn'])
    P.dma(ltri[:], d['ltri'])
    P.dma(utri[:], d['utri'])
    P.memset(onef[:], 1.0)
    for t in range(NT):
        self.hn_tile(hnT[:, :, t * 512:(t + 1) * 512], 0, t * 512)
    for t in range(NT):
        for c in range(8):
            P.mm(self.s2[0:16, :], wa[:, c, :], hnT[:, c, t * 512:(t + 1) * 512], start=(c == 0), stop=(c == 7))
        P.copy(alT[:, t * 512:(t + 1) * 512], self.s2[0:16, :])
    ogf = [sb(f"g_ogf{i}", [128, 256], F32) for i in range(NB)]

    gsets = [dict(eb=sb(f"g_eb4{i}", [128, 512], F32), enb=sb(f"g_enb4{i}", [128, 512], F32),
                  erb=sb(f"g_erb4{i}", [128, 512], F32), ebl=sb(f"g_ebl4{i}", [128, 4], F32)) for i in range(2)]
    zb4 = sb("g_zb4", [128, 512], F32)
    gg4 = zb4

    def groupprep(h, G, gs):
        t0g = G * 512
        Z = self.s2
        for cc in range(4):
            P.mm(Z[:, cc * 128:(cc + 1) * 128], alT[0:16, t0g + cc * 128:t0g + (cc + 1) * 128], wau[0:16, h * 128:(h + 1) * 128])
        yield
        for cc in range(4):
            P.tt(zb4[:, cc * 128:(cc + 1) * 128], Z[:, cc * 128:(cc + 1) * 128], ba[:, h * 128:(h + 1) * 128], ALU.add)
        yield
        P.act(zb4[:], zb4[:], AF.Exp, scale=-1.0)
        yield
        P.act(zb4[:], zb4[:], AF.Ln, bias=onef[:, 0:1])
        yield
        P.ts(gg4[:], zb4[:], -1.0 / 16.0, None, ALU.mult)
        yield
        P.mm(Z[:, :], ltri[:], gg4[:])
        yield
        P.act(gs['eb'][:], Z[:, :], AF.Exp)
        P.act(gs['enb'][:], Z[:, :], AF.Exp, scale=-1.0)
        yield
        P.mm(Z[:, :], utri[:], gg4[:])
        yield
        P.act(gs['erb'][:], Z[:, :], AF.Exp)
        yield
        for cc in range(4):
            P.mm(Z[:, cc * 128:(cc + 1) * 128], gg4[:, cc * 128:(cc + 1) * 128], onef[:])
        yield
        P.act(gs['ebl'][:], Z[:, 0:512:128], AF.Exp)
        yield

    def phaseA(h, i, b_):
        whb = wh[h % 2]
        t0 = i * 128
        gs = gsets[(h * 4 + i // 4) % 2]
        cs = slice((i % 4) * 128, (i % 4 + 1) * 128)
        A = self.s0
        Bp = self.s1[:, 0:256]
        W = self.big0
        for c in range(8):
            P.mm(A[:], hnT[:, c, t0:t0 + 128], whb[:, c, 0:512], start=(c == 0), stop=(c == 7))
        for c in range(8):
            P.mm(Bp, hnT[:, c, t0:t0 + 128], whb[:, c, 512:768], start=(c == 0), stop=(c == 7))
        yield
        P.copy(vv[b_][:], A[:, 256:512], eng='act')
        P.act(sr[b_][:], Bp, AF.Exp, scale=-1.0)
        P.stt(qk[b_][:, 0:128], A[:, 0:128], float(128 ** -0.5), gs['eb'][:, cs], ALU.mult, ALU.mult)
        P.tt(qk[b_][:, 128:256], A[:, 128:256], gs['enb'][:, cs], ALU.mult)
        yield
        P.tt(kh[b_][:], A[:, 128:256], gs['erb'][:, cs], ALU.mult)
        P.ts(sr[b_][:], sr[b_][:], 1.0, None, ALU.add)
        P.transpose(self.pst[:, 0:128], qk[b_][:, 0:128], self.identb[:])
        P.transpose(self.pst[:, 128:256], qk[b_][:, 128:256], self.identb[:])
        yield
        P.copy(qkT[b_][:], self.pst[:, 0:256])
        P.op('dve', lambda e, a=sr[b_][:]: e.reciprocal(a, a), reads=[sr[b_][:]], writes=[sr[b_][:]])
        yield
        P.mm(W[:, 0:128], qkT[b_][:, 128:256], qkT[b_][:, 0:128])
        P.tt(sr[b_][:], Bp, sr[b_][:], ALU.mult)
        yield
        P.tt(attm[b_][:], W[:, 0:128], ltri[:], ALU.mult)
        yield

    def phaseB(h, i, b_):
        t0 = i * 128
        Wo = self.big0[:, 512:768]
        Ws = self.big1[:, 0:256]
        Wt = self.big1[:, 512:768]
        P.mm(Wo, qkT[b_][:, 0:128], Sb[:], start=True, stop=False)
        P.mm(Wo, attm[b_][:], vv[b_][:], start=False, stop=True)
        P.mm(Ws, kh[b_][:], vv[b_][:])
        yield
        P.stt(Sf[:], Sf[:], gsets[(h * 4 + i // 4) % 2]['ebl'][:, (i % 4):(i % 4) + 1], Ws, ALU.mult, ALU.add)
        P.act(sq[b_][:], Wo, AF.Square)
        yield
        P.copy(Sb[:], Sf[:], eng='pool')
        P.op('dve', lambda e, o_=ss[b_][:], i_=sq[b_][:]: e.reduce_sum(o_, i_, axis=AX.X), reads=[sq[b_][:]], writes=[ss[b_][:]])
        yield
        P.act(ss[b_][:], ss[b_][:], AF.Ln, bias=self.epsc[:, 0:1], scale=1.0 / 256)
        yield
        P.act(ss[b_][:], ss[b_][:], AF.Exp, scale=-0.5)
        yield
        P.stt(og[b_][:], Wo, ss[b_][:, 0:1], gn[:, h * 256:(h + 1) * 256], ALU.mult, ALU.mult)
        yield
        P.tt(ogf[b_][:], og[b_][:], sr[b_][:], ALU.mult, eng='pool')
        yield
        for j in range(2):
            P.transpose(Wt[:, j * 128:(j + 1) * 128], ogf[b_][:, j * 128:(j + 1) * 128], self.identf[:])
        yield
        P.copy(oT[:, 2 * h:2 * h + 2, t0:t0 + 128], Wt.rearrange("p (j t) -> p j t", j=2), eng='act')
        yield

    def interleave(gens):
        gens = [g_ for g_ in gens if g_ is not None]
        while gens:
            for g_ in list(gens):
                try:
                    next(g_)
                except StopIteration:
                    gens.remove(g_)

    P.dma(wh[0][:], d['gla_wh'][0], eng='pool')
    seq = [(h, i) for h in range(4) for i in range(16)]
    interleave([groupprep(0, 0, gsets[0])])
    interleave([phaseA(0, 0, 0)])
    for n_, (h, i) in enumerate(seq):
        if i == 0:
            P.memset(Sf[:], 0.0, eng='dve')
            P.memset(Sb[:], 0.0, eng='dve')
            if h + 1 < 4:
                P.dma(wh[(h + 1) % 2][:], d['gla_wh'][h + 1], eng='pool')
        nxt = seq[n_ + 1] if n_ + 1 < len(seq) else None
        prep = None
        if i % 4 == 0:
            gidx = h * 4 + i // 4 + 1
            if gidx < 16:
                prep = groupprep(gidx // 4, gidx % 4, gsets[gidx % 2])
        interleave([phaseB(h, i, n_ % NB), phaseA(nxt[0], nxt[1], (n_ + 1) % NB) if nxt else None, prep])
    self.pop()
    self.push()
    wo0 = sb("g_wo0", [128, 8, 512], BF16)
    wo1 = sb("g_wo1", [128, 8, 512], BF16)
    P.dma(wo0[:], d['gla_wo'][:, :, 0:512], eng='pool')
    P.dma(wo1[:], d['gla_wo'][:, :, 512:1024], eng='pool')
    banks = [self.s0, self.s1, self.s2]
    n = 0
    for m in range(8):
        wb = wo0 if m < 4 else wo1
        mm_ = m % 4
        for t in range(NT):
            pb = banks[n % 3]
            n += 1
            for c in range(8):
                P.mm(pb[:], wb[:, c, mm_ * 128:(mm_ + 1) * 128], oT[:, c, t * 512:(t + 1) * 512], start=(c == 0), stop=(c == 7))
            xs = self.xT[:, m, t * 512:(t + 1) * 512]
            P.tt(xs, pb[:], xs, ALU.add)
    self.pop()
    self.pop()


K.gla = _gla


def _run_seq(self, si):
    self.load_x(si)
    st = self.stop
    if st == 'load':
        return self.final_store(si)
    if st == 'ffn0':
        self.ffn(0)
        return self.final_store(si)
    self.gla()
    if st == 'gla':
        return self.final_store(si)
    self.final_store(si)


K.run_seq = _run_seq


BIG = 30000.0
SCALE = 0.125


def _nsa(self):
    P, d = self.P, self.din
    sb = self.sb
    self.compute_rstd()
    self.push()
    oT = sb("n_oT", [128, 8, S], BF16)
    LK = sb("n_LK", [96, S], BF16)
    kwT = sb("n_kwT", [64, S], BF16)
    V1 = sb("n_V1", [128, 16, 2, 65], BF16)
    kcT = sb("n_kcT", [64, 128], BF16)
    vc = sb("n_vc", [128, 64], BF16)
    hnb = sb("n_hnb", [128, 8, 512], BF16)
    rc = sb("n_rc", [64, 512], F32)
    rs = sb("n_rs", [64, 512], F32)
    r1 = sb("n_r1", [64, 512], F32)
    r2 = sb("n_r2", [64, 512], F32)
    P.dma(LK[64:96, :], d['expand'], eng='pool')
    sc_banks = [self.s0, self.s1, self.s2]
    scn = [0]

    def scbank():
        b = sc_banks[scn[0] % 3]
        scn[0] += 1
        return b

    def rope_to(dst, pa, pb_):
        P.tt(r1[:], pa, rc[:], ALU.mult)
        P.tt(r2[:], pb_, rs[:], ALU.mult)
        P.tt(dst, r1[:], r2[:], ALU.add, eng='pool')

    for hk in range(4):
        self.push()
        wkv = sb("n_wkv", [128, 8, 512], BF16)
        rawT = sb("n_rawT", [64, 2, S], BF16)
        w1bs = [sb(f"n_w1b{i}", [64, 32, 256], BF16) for i in range(2)]
        w2bs = [sb(f"n_w2b{i}", [128, 2, 64], BF16) for i in range(2)]
        pefs = [sb(f"n_pef{i}", [64, 32], F32) for i in range(2)]
        hnb2 = sb("n_hnb2", [128, 8, 512], BF16)
        pe2 = sb("n_pe2", [64, 32, 2], BF16)
        cb = sb("n_cb", [128, 2], F32)
        hx = sb("n_hx", [128, 127], F32)
        hx2 = sb("n_hx2", [128, 127], F32)
        hidT = sb("n_hidT", [128, 2, 127], BF16)
        P.dma(wkv[:], d['nsa_wkv'][hk], eng='pool')
        for kind in range(2):
            P.dma(w1bs[kind][:], d['cmp_w1'][kind], eng='pool')
            P.dma(w2bs[kind][:], d['cmp_w2'][kind], eng='pool')
            P.dma(pefs[kind][:], d['cmp_peT'][kind])
        P.memset(V1[:], 1.0, eng='dve')
        hnb_q = hnb
        for tt in range(NT):
            t0 = tt * 512
            hnb = hnb_q if tt % 2 == 0 else hnb2
            self.hn_tile(hnb, 4, t0)
            P.dma(rc[:], d['ropec'][:, t0:t0 + 512])
            P.dma(rs[:], d['ropes'][:, t0:t0 + 512])
            pbs = []
            for u in range(6):
                pb = scbank() if u < 2 else [self.big0[:, 0:512], self.big0[:, 512:1024], self.big1[:, 0:512], self.big1[:, 512:1024]][u - 2]
                for c in range(8):
                    P.mm(pb[0:64, 0:512], wkv[:, c, u * 64:(u + 1) * 64], hnb[:, c, :], start=(c == 0), stop=(c == 7))
                pbs.append(pb)
                if u < 2:
                    P.copy(rawT[:, u, t0:t0 + 512], pb[0:64, 0:512], eng='act')
            rope_to(LK[0:64, t0:t0 + 512], pbs[2][0:64, 0:512], pbs[3][0:64, 0:512])
            rope_to(kwT[0:64, t0:t0 + 512], pbs[4][0:64, 0:512], pbs[5][0:64, 0:512])
            for sub in range(4):
                pb = scbank()
                for c in range(8):
                    P.mm(pb[:, 0:128], hnb[:, c, sub * 128:(sub + 1) * 128], wkv[:, c, 384:512], start=(c == 0), stop=(c == 7))
                P.copy(V1[:, tt * 4 + sub, :, 0:64], pb[:, 0:128].rearrange("p (b e) -> p b e", b=2), eng='act')
        hnb = hnb_q
        for kind in range(2):
            w1b = w1bs[kind]; w2b = w2bs[kind]; pef = pefs[kind]
            P.copy(pe2[:, :, 0], pef[:])
            P.copy(pe2[:, :, 1], pef[:])
            for ht in range(2):
                pb = scbank()
                for j in range(32):
                    P.mm(pb[:, 0:2], w1b[:, j, ht * 128:(ht + 1) * 128], pe2[:, j, :], start=(j == 0), stop=(j == 31))
                P.copy(cb[:, ht:ht + 1], pb[:, 0:1])
                pb = scbank()
                for j in range(32):
                    P.mm(pb[:, 0:127], w1b[:, j, ht * 128:(ht + 1) * 128], rawT[0:64, kind, j:j + 2017:16],
                         start=(j == 0), stop=(j == 31))
                P.act(hx[:], pb[:, 0:127], AF.Identity, bias=cb[:, ht:ht + 1])
                P.tt(hx2[:], hx[:], hx[:], ALU.mult)
                P.ts(hx2[:], hx2[:], 0.044715, 1.0, ALU.mult, ALU.add)
                P.tt(hx2[:], hx2[:], hx[:], ALU.mult)
                P.act(hx2[:], hx2[:], AF.Exp, scale=-1.5957691216057308)
                P.ts(hx2[:], hx2[:], 1.0, None, ALU.add)
                P.op('dve', lambda e, a=hx2[:]: e.reciprocal(a, a), reads=[hx2[:]], writes=[hx2[:]])
                P.tt(hidT[:, ht, :], hx[:], hx2[:], ALU.mult)
            pb = scbank()
            if kind == 0:
                for ht in range(2):
                    P.mm(pb[0:64, 0:127], w2b[:, ht, :], hidT[:, ht, :], start=(ht == 0), stop=(ht == 1))
                P.copy(kcT[:, 0:127], pb[0:64, 0:127])
            else:
                for ht in range(2):
                    P.mm(pb[0:127, 0:64], hidT[:, ht, :], w2b[:, ht, :], start=(ht == 0), stop=(ht == 1))
                P.copy(vc[0:127, :], pb[0:127, 0:64])
        self.pop()
        self.push()
        wq = sb("n_wq", [128, 8, 512], BF16)
        wg = sb("n_wg", [128, 8, 12], BF16)
        RQs = [sb(f"n_RQ{i}", [96, 4, 512], BF16) for i in range(2)]
        qnat = sb("n_qnat", [64, 4, 512], BF16)
        band = sb("n_band", [128, 8, 512], BF16)
        cmask = sb("n_cmask", [128, 512], BF16)
        selA = sb("n_selA", [128, 4, 32], F32)
        selB = sb("n_selB", [128, 4, 32], F32)
        wov = sb("n_wov", [128, 32], BF16)
        NPT = 8
        pT = [sb(f"n_pT{i}", [128, 512], BF16) for i in range(NPT)]
        cpt = [sb(f"n_cpt{i}", [128, 512], BF16) for i in range(2)]
        dens = [sb(f"n_den{i}", [128, 512], F32) for i in range(2)]
        pns = [sb(f"n_pn{i}", [128, 512], BF16) for i in range(2)]
        gts = [sb(f"n_gt{i}", [128, 4, 12], F32) for i in range(2)]
        otoks = [sb(f"n_otok{i}", [128, 4, 256], F32) for i in range(2)]
        otb = sb("n_otb", [128, 4, 256], BF16)
        impa = sb("n_impa", [128, 4, 32], F32)
        imp2 = sb("n_imp2", [128, 32], F32)
        imp3 = sb("n_imp3", [128, 32], F32)
        m8 = sb("n_m8", [128, 16], F32)
        msel = sb("n_msel", [128, 32], F32)
        negm = sb("n_negm", [128, 4, 96], BF16)
        rden = sb("n_rden", [128, 4], F32)
        fac = sb("n_fac", [128, 4], F32)
        rcs = sb("n_rcs", [128, 512], F32)
        r12s = [sb(f"n_r12{i}", [128, 512], F32) for i in range(2)]
        r2ss = [sb(f"n_r2s{i}", [64, 512], F32) for i in range(2)]
        P.dma(wq[:], d['nsa_wq'][hk], eng='pool')
        P.dma(wg[:], d['nsa_wg'][hk], eng='pool')
        P.dma(band[:], d['band'], eng='pool')
        P.dma(wov[0:127, :], d['wov'], eng='pool')
        P.memset(negm[:], 0.0, eng='dve')
        pti = [0]

        def next_pT():
            b = pT[pti[0] % NPT]
            pti[0] += 1
            return b

        B1 = [self.big1[:, 0:512], self.big1[:, 512:1024]]

        def prologue(qt):
            q0 = qt * 512
            qb = q0 // 128
            RQ = RQs[qt % 2]; gt = gts[qt % 2]; otok = otoks[qt % 2]
            self.hn_tile(hnb, 1, q0)
            P.dma(rcs[0:64, :], d['ropec'][:, q0:q0 + 512])
            P.dma(rcs[64:128, :], d['ropes'][:, q0:q0 + 512])
            P.dma(cmask[0:127, :], d['cmpmask'][:, q0:q0 + 512], eng='pool')
            P.dma(selA[:], d['selA'][:, qb:qb + 4, :])
            P.dma(selB[:], d['selB'][:, qb:qb + 4, :])
            yield
            for sub in range(4):
                pb = B1[sub % 2]
                for c in range(8):
                    P.mm(pb[:, 0:12], hnb[:, c, sub * 128:(sub + 1) * 128], wg[:, c, :], start=(c == 0), stop=(c == 7))
                P.act(gt[:, sub, :], pb[:, 0:12], AF.Exp, scale=-1.0)
                yield
            P.ts(gt[:], gt[:], 1.0, None, ALU.add)
            P.op('dve', lambda e, a=gt[:]: e.reciprocal(a, a), reads=[gt[:]], writes=[gt[:]])
            P.memset(impa[:], 0.0, eng='dve')

            def gchain(g):
                pa = B1[g % 2]
                r12 = r12s[g % 2]; r2s = r2ss[g % 2]
                for c in range(8):
                    P.mm(pa[:, :], wq[:, c, g * 128:(g + 1) * 128], hnb[:, c, :], start=(c == 0), stop=(c == 7))
                yield
                P.copy(qnat[:, g, :], pa[0:64, :], eng='act')
                P.tt(r12[:], pa[:, :], rcs[:], ALU.mult)
                yield
                P.copy(r2s[:], r12[64:128, :], eng='pool')
                yield
                P.tt(RQ[0:64, g, :], r12[0:64, :], r2s[:], ALU.add, eng='pool')
                yield
                pb = B1[g % 2]
                pt = cpt[g % 2]; den = dens[g % 2]; pn = pns[g % 2]
                P.mm(pb[0:127, :], kcT[:, 0:127], qnat[:, g, :])
                yield
                P.act(pt[0:127, :], pb[0:127, :], AF.Exp, scale=SCALE)
                yield
                P.tt(pt[0:127, :], pt[0:127, :], cmask[0:127, :], ALU.mult, eng='pool')
                yield
                P.mm(pb[:, :], self.onesb[0:127, :], pt[0:127, :])
                yield
                P.ts(den[:], pb[:, :], 1e-30, None, ALU.add)
                yield
                P.op('dve', lambda e, a=den[:]: e.reciprocal(a, a), reads=[den[:]], writes=[den[:]])
                yield
                P.tt(pn[0:127, :], pt[0:127, :], den[0:127, :], ALU.mult, eng='pool')
                yield
                for sub in range(4):
                    P.mm(pb[:, sub * 96:sub * 96 + 64], pn[0:127, sub * 128:(sub + 1) * 128], vc[0:127, :])
                    P.mm(pb[:, sub * 96 + 64:sub * 96 + 96], pn[0:127, sub * 128:(sub + 1) * 128], wov[0:127, :])
                yield
                pv = pb[:, 0:384].rearrange("p (s c) -> p s c", c=96)
                for sub in range(4):
                    P.ts(otok[:, sub, g * 64:(g + 1) * 64], pv[:, sub, 0:64], gt[:, sub, g:g + 1], None, ALU.mult)
                P.tt(impa[:], pv[:, :, 64:96], impa[:], ALU.add)
                yield

            for gp in range(2):
                ga, gb = gchain(2 * gp), gchain(2 * gp + 1)
                alive = [ga, gb]
                while alive:
                    for g_ in list(alive):
                        try:
                            next(g_)
                        except StopIteration:
                            alive.remove(g_)
                    yield
            for sub in range(4):
                P.tt(imp2[:], impa[:, sub, :], selA[:, sub, :], ALU.mult)
                P.tt(imp2[:], imp2[:], selB[:, sub, :], ALU.add)
                yield
                P.op('dve', lambda e: e.max(m8[:, 0:8], imp2[:]), reads=[imp2[:]], writes=[m8[:, 0:8]])
                P.op('dve', lambda e: e.match_replace(imp3[:], m8[:, 0:8], imp2[:], -1e30), reads=[m8[:, 0:8], imp2[:]], writes=[imp3[:]])
                yield
                P.op('dve', lambda e: e.max(m8[:, 8:16], imp3[:]), reads=[imp3[:]], writes=[m8[:, 8:16]])
                P.ts(msel[:], imp2[:], m8[:, 15:16], None, ALU.is_ge)
                yield
                P.ts(negm[:, sub, 64:96], msel[:], 1.0, BIG, ALU.subtract, ALU.mult)
                yield
                P.transpose(self.pst[0:96, 512 + sub * 128:512 + (sub + 1) * 128], negm[:, sub, :], self.identb[:])
                yield
            for g in range(4):
                P.copy(RQ[64:96, g, :], self.pst[64:96, 512:1024], eng='dve')
            yield

        def mainloop(qt):
            q0 = qt * 512
            qb = q0 // 128
            RQ = RQs[qt % 2]; gt = gts[qt % 2]; otok = otoks[qt % 2]
            tiles = []
            for br in range(2):
                for g in range(4):
                    kts = list(range(0, qb + 4)) if br == 0 else list(range(max(0, qb - 4), qb + 4))
                    for ii, kt in enumerate(kts):
                        tiles.append((br, g, kt, ii == 0, ii == len(kts) - 1))
            LOOK = 4
            pts = {}
            main_banks = [self.s0, self.s1, self.s2]

            def st_score(i):
                br, g, kt, first, last = tiles[i]
                r = kt - qb + 4
                pb = main_banks[i % len(main_banks)]
                masked = (br == 1 or r >= 4)
                if br == 0:
                    P.mm(pb[:, 0:512], LK[0:96, kt * 128:(kt + 1) * 128], RQ[0:96, g, :])
                else:
                    P.mm(pb[:, 0:512], kwT[0:64, kt * 128:(kt + 1) * 128], RQ[0:64, g, :])
                pt = next_pT()
                pts[i] = pt
                P.act(pt[:], pb[:, 0:512], AF.Exp, scale=SCALE)
                if masked:
                    P.tt(pt[:], pt[:], band[:, r, :], ALU.mult)

            def st_pv(i):
                br, g, kt, first, last = tiles[i]
                r = kt - qb + 4
                O = self.big0[:, 0:260] if (g % 2 == 0) else self.big0[:, 512:772]
                pt = pts.pop(i)
                if first:
                    P.memset(O, 0.0, eng='dve')
                for sub in range(4):
                    if r - sub > 4 or (br == 1 and r - sub < 0):
                        continue
                    P.mm(O[:, sub * 65:(sub + 1) * 65], pt[:, sub * 128:(sub + 1) * 128], V1[:, kt, br, :],
                         start=False, stop=False, skip=True)
                if last:
                    Ov = O.rearrange("p (s c) -> p s c", c=65)
                    P.op('dve', lambda e, o_=rden[:], i_=Ov[:, :, 64]: e.reciprocal(o_, i_), reads=[O], writes=[rden[:]])
                    P.tt(fac[:], rden[:], gt[:, :, (br + 1) * 4 + g], ALU.mult)
                    for sub in range(4):
                        os_ = otok[:, sub, g * 64:(g + 1) * 64]
                        P.stt(os_, Ov[:, sub, 0:64], fac[:, sub:sub + 1], os_, ALU.mult, ALU.add)

            for i in range(len(tiles) + LOOK):
                if i < len(tiles):
                    st_score(i)
                if i - LOOK >= 0:
                    st_pv(i - LOOK)
                yield
            P.copy(otb[:], otok[:], eng='pool')
            for sub in range(4):
                for j in range(2):
                    P.transpose(self.pst[:, j * 128:(j + 1) * 128], otb[:, sub, j * 128:(j + 1) * 128], self.identb[:])
                P.copy(oT[:, 2 * hk:2 * hk + 2, q0 + sub * 128:q0 + (sub + 1) * 128],
                       self.pst[:, 0:256].rearrange("p (j t) -> p j t", j=2), eng='act')
            yield

        def interleave(gens):
            gens = [g_ for g_ in gens if g_ is not None]
            while gens:
                for g_ in list(gens):
                    try:
                        next(g_)
                    except StopIteration:
                        gens.remove(g_)

        order = [3, 2, 1, 0]
        interleave([prologue(order[0])])
        for oi, qt in enumerate(order):
            interleave([mainloop(qt), prologue(order[oi + 1]) if oi + 1 < NT else None])
        self.pop()
    self.push()
    wo0 = sb("n_wo0", [128, 8, 512], BF16)
    wo1 = sb("n_wo1", [128, 8, 512], BF16)
    P.dma(wo0[:], d['nsa_wo2'][:, :, 0:512], eng='pool')
    P.dma(wo1[:], d['nsa_wo2'][:, :, 512:1024], eng='pool')
    n = 0
    for m in range(8):
        wb = wo0 if m < 4 else wo1
        mm_ = m % 4
        for t in range(NT):
            pb = sc_banks[n % 3]
            n += 1
            for c in range(8):
                P.mm(pb[:], wb[:, c, mm_ * 128:(mm_ + 1) * 128], oT[:, c, t * 512:(t + 1) * 512], start=(c == 0), stop=(c == 7))
            xs = self.xT[:, m, t * 512:(t + 1) * 512]
            P.tt(xs, pb[:], xs, ALU.add)
    self.pop()
    self.pop()


K.nsa = _nsa


def _run_seq(self, si):
    self.load_x(si)
    st = self.stop
    if st == 'load':
        return self.final_store(si)
    if st == 'ffn0':
        self.ffn(0)
        return self.final_store(si)
    if st == 'nsa':
        self.nsa()
        return self.final_store(si)
    self.gla()
    if st == 'gla':
        return self.final_store(si)
    self.ffn(0)
    self.nsa()
    self.ffn(1)
    self.final_store(si)


K.run_seq = _run_seq


N_CORES = 8


def build_program(nseq, shapes, stop=None):
    from contextlib import ExitStack
    nc = bass.Bass("TRN2", target_bir_lowering=False)
    with ExitStack() as stack:
        P = Prog(nc)
        k = K(nc, P, stack, nseq, shapes, stop)
        k.setup()
        for si in range(nseq):
            k.run_seq(si)
        P.emit(stack)
    return nc, P


def run(inputs, n_cores=N_CORES, stop=None, trace=False):
    x = np.ascontiguousarray(inputs['x'], dtype=np.float32)
    B = x.shape[0]
    nseq = B // n_cores
    shared = host_prep(inputs)
    shared.update(host_consts())
    shapes = {k: v.shape for k, v in shared.items()}
    shapes['x'] = (nseq, S, D)
    nc, P = build_program(nseq, shapes, stop)
    in_maps = []
    for c in range(n_cores):
        m = dict(shared)
        m['x'] = x[c * nseq:(c + 1) * nseq]
        in_maps.append(m)
    res = run_bass_kernel_spmd(nc, in_maps, core_ids=list(range(n_cores)), trace=trace)
    out = np.concatenate([r['out'] for r in res.results], axis=0)
    return out, res, P


def kernel(**inputs):
    out, _, _ = run(inputs)
    return out.astype(np.float32)
```

```python
import numpy as np
import concourse.bass as bass
import concourse.mybir as mybir
from concourse.bass_utils import run_bass_kernel_spmd

F32 = mybir.dt.float32
BF16 = mybir.dt.bfloat16
AF = mybir.ActivationFunctionType
ALU = mybir.AluOpType
AX = mybir.AxisListType

SEM_CH = 16000
N_DMA_SEMS = 24


def _region(ap):
    t = ap.tensor
    cls = type(t).__name__
    if 'DRam' in cls or 'Dram' in cls or 'DRAM' in cls:
        return None
    pairs = ap.ap
    shape = t.shape
    pstride = 1
    for s in shape[1:]:
        pstride *= s
    off = int(ap.offset)
    p0 = off // pstride
    f0 = off % pstride
    if pairs[0][0] == pstride or pairs[0][1] == 1:
        pc = pairs[0][1]
        rest = pairs[1:]
    else:
        pc = 1
        rest = pairs
    ext = 0
    for st, cnt in rest:
        ext += (cnt - 1) * abs(st)
    f1 = f0 + ext + 1
    if 'PSum' in cls or 'Psum' in cls or 'PSUM' in cls:
        be = 2048 // (2 if 'bfloat16' in str(ap.dtype) or 'float16' in str(ap.dtype) else 4)
        return (t.name, 0, 128, (f0 // be) * be, ((f1 + be - 1) // be) * be, True)
    return (t.name, p0, p0 + pc, f0, f1, False)


def _overlap(a, b):
    return a[1] < b[2] and b[1] < a[2] and a[3] < b[4] and b[3] < a[4]


def _contains(a, b):
    return a[1] <= b[1] and b[2] <= a[2] and a[3] <= b[3] and b[4] <= a[4]


class Prog:
    ENGS = ('pe', 'act', 'dve', 'pool', 'sp')

    def __init__(self, nc):
        self.nc = nc
        self.ops = []
        self.hist = {}
        self.bar_pending = set()
        self.bar_deps = set()
        self.bar_deps_prev = set()
        self.bar_start = 0

    def barrier(self):
        last = {}
        dmas = set()
        for o in self.ops[self.bar_start:]:
            if o['dma']:
                dmas.add(o['idx'])
            else:
                last[o['eng']] = o['idx']
        self.bar_deps = set(last.values()) | dmas | set(self.bar_deps_prev)
        self.bar_deps_prev = set(last.values())
        self.bar_pending = set(self.ENGS)
        self.bar_start = len(self.ops)
        self.hist = {}

    def op(self, eng, fn, reads=(), writes=(), dma=False):
        idx = len(self.ops)
        deps = set()
        if eng in self.bar_pending:
            deps |= self.bar_deps
            self.bar_pending.discard(eng)
        rr = [r for r in (_region(a) for a in reads) if r is not None]
        ww = [r for r in (_region(a) for a in writes) if r is not None]
        ww = ww + [r for r in rr if r[5]]
        rr = [r for r in rr if not r[5]]
        for r in rr:
            for rec in self.hist.get(r[0], ()):
                if rec[6] and _overlap(r, rec):
                    deps.add(rec[5])
        for w in ww:
            for rec in self.hist.get(w[0], ()):
                if _overlap(w, rec):
                    deps.add(rec[5])
        deps.discard(idx)
        for w in ww:
            lst = self.hist.setdefault(w[0], [])
            lst[:] = [rec for rec in lst if not _contains(w, rec)]
            lst.append((w[0], w[1], w[2], w[3], w[4], idx, True, eng, dma))
        for r in rr:
            lst = self.hist.setdefault(r[0], [])
            if not dma:
                lst[:] = [rec for rec in lst
                          if not ((not rec[6]) and rec[7] == eng and not rec[8] and _contains(r, rec))]
            lst.append((r[0], r[1], r[2], r[3], r[4], idx, False, eng, dma))
        self.ops.append(dict(eng=eng, fn=fn, deps=deps, dma=dma, idx=idx))
        return idx

    def emit(self, stack):
        nc = self.nc
        ops = self.ops
        signal = [False] * len(ops)
        for o in ops:
            for d in o['deps']:
                od = ops[d]
                if od['eng'] == 'pe' and o['eng'] == 'pe' and not od['dma'] and not o['dma']:
                    continue
                signal[d] = True
        eng_count = {e: 0 for e in self.ENGS}
        eng_sems = {e: [] for e in self.ENGS}
        dma_sems = [stack.enter_context(nc.semaphore(f"dsem{i}")) for i in range(N_DMA_SEMS)]
        dma_tot = [0] * N_DMA_SEMS
        dma_last = [None] * N_DMA_SEMS
        n_sw = 4
        n_hw = N_DMA_SEMS - n_sw
        dma_rr = {'sw': 0, 'hw': 0}
        token = [None] * len(ops)
        extra_dep = [None] * len(ops)
        for o in ops:
            i = o['idx']
            if o['dma']:
                kind = 'sw' if o['eng'] == 'pool' else 'hw'
                k = (dma_rr[kind] % n_sw) + n_hw if kind == 'sw' else (dma_rr[kind] % n_hw)
                dma_rr[kind] += 1
                extra_dep[i] = dma_last[k]
                dma_tot[k] += 16
                token[i] = (dma_sems[k], dma_tot[k], 16)
                dma_last[k] = i
            elif signal[i]:
                e = o['eng']
                c = eng_count[e]
                ep = c // SEM_CH
                if ep >= len(eng_sems[e]):
                    eng_sems[e].append(stack.enter_context(nc.semaphore(f"s_{e}{ep}")))
                token[i] = (eng_sems[e][ep], c % SEM_CH + 1, 1)
                eng_count[e] = c + 1
        self.n_signal = dict(eng_count)
        block = stack.enter_context(nc.Block())

        def run_engine(e, eng_obj):
            waited = {}
            my_dma = set()
            for o in ops:
                if o['eng'] != e:
                    continue
                i = o['idx']
                deps = set(o['deps'])
                if extra_dep[i] is not None:
                    deps.add(extra_dep[i])
                need = {}
                for d in deps:
                    od = ops[d]
                    if od['eng'] == 'pe' and e == 'pe' and not od['dma'] and not o['dma']:
                        continue
                    sem, val, _ = token[d]
                    key = id(sem)
                    if waited.get(key, 0) >= val:
                        continue
                    if key not in need or need[key][1] < val:
                        need[key] = (sem, val)
                for key, (sem, val) in need.items():
                    eng_obj.wait_ge(sem, val)
                    waited[key] = val
                ins = o['fn'](eng_obj)
                if token[i] is not None:
                    sem, val, inc = token[i]
                    ins.then_inc(sem, inc)
                    if o['dma']:
                        my_dma.add(i)
            fin = {}
            for i in my_dma:
                sem, val, _ = token[i]
                key = id(sem)
                if key not in fin or fin[key][1] < val:
                    fin[key] = (sem, val)
            for key, (sem, val) in fin.items():
                if waited.get(key, 0) < val:
                    eng_obj.wait_ge(sem, val)

        @block.tensor
        def _(pe):
            run_engine('pe', pe)

        @block.scalar
        def _(act):
            run_engine('act', act)

        @block.vector
        def _(dve):
            run_engine('dve', dve)

        @block.gpsimd
        def _(pool):
            run_engine('pool', pool)

        @block.sync
        def _(sp):
            run_engine('sp', sp)

    def dma(self, out, in_, eng='sp'):
        return self.op(eng, lambda e: e.dma_start(out=out, in_=in_), reads=[in_], writes=[out], dma=True)

    def mm(self, out, lhsT, rhs, start=True, stop=True, skip=False):
        rd = [lhsT, rhs] + ([] if start else [out])
        if skip:
            return self.op('pe', lambda e: e.matmul(out, lhsT, rhs, start=start, stop=stop, skip_group_check=True), reads=rd, writes=[out])
        return self.op('pe', lambda e: e.matmul(out, lhsT, rhs, start=start, stop=stop), reads=rd, writes=[out])

    def transpose(self, out, in_, ident):
        return self.op('pe', lambda e: e.transpose(out, in_, ident), reads=[in_, ident], writes=[out])

    def act(self, out, in_, func, bias=None, scale=None, accum_out=None, eng='act'):
        kw = {}
        rd = [in_]
        wr = [out]
        if bias is not None:
            kw['bias'] = bias
            if not isinstance(bias, (int, float)):
                rd.append(bias)
        if scale is not None:
            kw['scale'] = scale
            if not isinstance(scale, (int, float)):
                rd.append(scale)
        if accum_out is not None:
            kw['accum_out'] = accum_out
            wr.append(accum_out)
        return self.op(eng, lambda e: e.activation(out, in_, func, **kw), reads=rd, writes=wr)

    def tt(self, out, in0, in1, op, eng='dve'):
        return self.op(eng, lambda e: e.tensor_tensor(out, in0, in1, op), reads=[in0, in1], writes=[out])

    def ts(self, out, in0, s1, s2, op0, op1=None, eng='dve', accum_out=None):
        rd = [in0] + [s for s in (s1, s2) if s is not None and not isinstance(s, (int, float))]
        wr = [out] + ([accum_out] if accum_out is not None else [])
        kw = {}
        if op1 is not None:
            kw['op1'] = op1
        if accum_out is not None:
            kw['accum_out'] = accum_out
        return self.op(eng, lambda e: e.tensor_scalar(out, in0, s1, s2, op0, **kw), reads=rd, writes=wr)

    def stt(self, out, in0, scalar, in1, op0, op1, eng='dve'):
        rd = [in0, in1] + ([] if isinstance(scalar, (int, float)) else [scalar])
        return self.op(eng, lambda e: e.scalar_tensor_tensor(out, in0, scalar, in1, op0, op1), reads=rd, writes=[out])

    def copy(self, out, in_, eng='dve'):
        if eng == 'act':
            return self.op(eng, lambda e: e.activation(out, in_, AF.Copy), reads=[in_], writes=[out])
        return self.op(eng, lambda e: e.tensor_copy(out, in_), reads=[in_], writes=[out])

    def memset(self, ap, val, eng='pool'):
        return self.op(eng, lambda e: e.memset(ap, val), writes=[ap])


S = 2048
D = 1024
NT = 4
EPS = 1e-6
FF = 2816
NPAIR = 22
FGROUPS = [list(range(i, min(i + 4, NPAIR))) for i in range(0, NPAIR, 4)]
WINS = [(0, 684, 0), (682, 684, 684), (1364, 684, 1366)]


def host_prep(inp):
    f = lambda a: np.ascontiguousarray(a, dtype=np.float32)
    o = {}
    def rows_tiled(w):
        K, N = w.shape
        return f(w.reshape(K // 128, 128, N).transpose(1, 0, 2))
    gains = np.stack([inp['norm_mix'][0], inp['norm_mix'][1], inp['norm_ffn'][0], inp['norm_ffn'][1],
                      inp['kv_norm'], inp['norm_final']], 0)
    o['gains'] = f(gains.reshape(6, 8, 128).transpose(2, 0, 1))
    for l in range(2):
        wu = inp['ffn_w_up'][l]
        pairs = []
        for j in range(NPAIR):
            pairs.append(np.concatenate([wu[:, j * 128:(j + 1) * 128], wu[:, FF + j * 128:FF + (j + 1) * 128]], 1))
        o[f'wup{l}'] = f(np.stack([rows_tiled(p) for p in pairs], 0))
        cw = inp['ffn_conv_w'][l]; cb = inp['ffn_conv_b'][l]
        par = np.stack([cw[0], cw[1], cw[2], cb], -1)
        par = par.reshape(2, NPAIR, 128, 4).transpose(2, 1, 0, 3)
        o[f'cpar{l}'] = f(par)
        o[f'wdn{l}'] = rows_tiled(inp['ffn_w_down'][l])
    wi = inp['gla_w_in'][0]
    hw = []
    for h in range(4):
        hw.append(np.concatenate([wi[:, h * 128:(h + 1) * 128], wi[:, 512 + h * 128:512 + (h + 1) * 128],
                                  wi[:, 1024 + h * 256:1024 + (h + 1) * 256], wi[:, 2048 + h * 256:2048 + (h + 1) * 256]], 1))
    o['gla_wh'] = f(np.stack([rows_tiled(w) for w in hw], 0))
    o['gla_wa'] = rows_tiled(wi[:, 3072:3088])
    o['gla_wau'] = f(inp['gla_w_alpha_up'][0])
    o['gla_ba'] = f(np.tile(inp['gla_b_alpha'][0][None, :], (128, 1)))
    o['gla_gn'] = f(np.tile(inp['gla_norm'][0].reshape(1, 1024), (128, 1)))
    o['gla_wo'] = rows_tiled(inp['gla_w_o'][0])
    wkv = inp['nsa_w_kv']; wq = inp['nsa_w_in'][0]
    sw = lambda w: np.concatenate([w[:, 32:64], w[:, 0:32]], 1)
    kvs, qs, gs = [], [], []
    for hk in range(4):
        c = lambda kind: wkv[:, kind * 256 + hk * 64: kind * 256 + (hk + 1) * 64]
        kvs.append(rows_tiled(np.concatenate([c(0), c(1), c(2), sw(c(2)), c(4), sw(c(4)), c(3), c(5)], 1)))
        ql = []
        for g in range(4):
            h = hk * 4 + g
            ql += [wq[:, h * 64:(h + 1) * 64], sw(wq[:, h * 64:(h + 1) * 64])]
        qs.append(rows_tiled(np.concatenate(ql, 1)))
        gcols = [1024 + b * 16 + hk * 4 + g for b in range(3) for g in range(4)]
        gs.append(rows_tiled(wq[:, gcols]))
    o['nsa_wkv'] = f(np.stack(kvs, 0)); o['nsa_wq'] = f(np.stack(qs, 0)); o['nsa_wg'] = f(np.stack(gs, 0))
    o['nsa_wo2'] = rows_tiled(inp['nsa_w_o'][0])
    o['cmp_w1'] = f(np.stack([inp['cmp_k_w1'].reshape(32, 64, 256).transpose(1, 0, 2),
                              inp['cmp_v_w1'].reshape(32, 64, 256).transpose(1, 0, 2)], 0))
    o['cmp_w2'] = f(np.stack([inp['cmp_k_w2'].reshape(2, 128, 64).transpose(1, 0, 2),
                              inp['cmp_v_w2'].reshape(2, 128, 64).transpose(1, 0, 2)], 0))
    o['cmp_peT'] = f(np.stack([inp['cmp_pe_k'].T, inp['cmp_pe_v'].T], 0))
    return o


def host_consts():
    c = {}
    c['ident'] = np.eye(128, dtype=np.float32)
    s = np.arange(128)
    c['ltri'] = (s[:, None] <= s[None, :]).astype(np.float32)
    c['utri'] = (s[:, None] > s[None, :]).astype(np.float32)
    pos = np.arange(S, dtype=np.float32)
    inv = (10000.0 ** (-np.arange(32, dtype=np.float32) / 32)).astype(np.float32)
    ang = pos[None, :] * inv[:, None]
    cos = np.cos(ang).astype(np.float32); sin = np.sin(ang).astype(np.float32)
    c['ropec'] = np.concatenate([cos, cos], 0)
    c['ropes'] = np.concatenate([-sin, sin], 0)
    n = np.arange(127)
    c['cmpmask'] = ((n[:, None] * 16 + 31) <= np.arange(S)[None, :]).astype(np.float32)
    c0 = n[:, None] * 16; s0 = np.arange(32)[None, :] * 64
    ov = np.clip(np.minimum(c0 + 32, s0 + 64) - np.maximum(c0, s0), 0, None)
    c['wov'] = (ov / 32).astype(np.float32)
    blk = np.arange(32)[None, :]; cur = (np.arange(S) // 64)[:, None]
    forced = (blk == 0) | (blk == cur) | (blk == cur - 1)
    A = np.where((blk > cur) | forced, 0.0, 1.0)
    B = np.where(blk > cur, -1.0, np.where(forced, 1e4, 0.0))
    c['selA'] = A.reshape(16, 128, 32).transpose(1, 0, 2).astype(np.float32).copy()
    c['selB'] = B.reshape(16, 128, 32).transpose(1, 0, 2).astype(np.float32).copy()
    c['expand'] = (np.arange(32)[:, None] == (np.arange(S) // 64)[None, :]).astype(np.float32)
    kp = np.arange(128)[:, None, None]; rel = np.arange(8)[None, :, None]; qq = np.arange(512)[None, None, :]
    dist = qq - (rel * 128 - 512 + kp)
    c['band'] = ((dist >= 0) & (dist < 512)).astype(np.float32)
    return {k: np.ascontiguousarray(v, dtype=np.float32) for k, v in c.items()}


class K:
    def __init__(self, nc, P, stack, nseq, shapes, stop=None):
        self.nc, self.P, self.stack, self.nseq, self.stop = nc, P, stack, nseq, stop
        self.din = {}
        for name, shp in shapes.items():
            self.din[name] = nc.dram_tensor(name, list(shp), F32, kind="ExternalInput").ap()
        self.out = nc.dram_tensor("out", [nseq, S, D], F32, kind="ExternalOutput").ap()
        self.scopes = []

    def sb(self, name, shape, dt, scoped=True):
        st = self.scopes[-1] if (scoped and self.scopes) else self.stack
        self.uid = getattr(self, 'uid', 0) + 1
        return st.enter_context(self.nc.sbuf_tensor(f"sb{self.uid}_{name}", shape, dt))

    def ps(self, name, shape, dt):
        return self.stack.enter_context(self.nc.psum_tensor("ps_" + name, shape, dt))

    def push(self):
        from contextlib import ExitStack
        es = ExitStack()
        self.scopes.append(es)

    def pop(self):
        self.P.barrier()
        self.scopes.pop().close()

    def setup(self):
        P, d = self.P, self.din
        self.xT = self.sb("xT", [128, 8, S], F32, scoped=False)
        self.rstd = self.sb("rstd", [128, S], F32, scoped=False)
        self.identf = self.sb("identf", [128, 128], F32, scoped=False)
        self.identb = self.sb("identb", [128, 128], BF16, scoped=False)
        self.onesb = self.sb("onesb", [128, 128], BF16, scoped=False)
        self.gains = self.sb("gains", [128, 6, 8], F32, scoped=False)
        self.epsc = self.sb("epsc", [128, 1], F32, scoped=False)
        self.hntmp = self.sb("hntmp", [128, 2, 512], F32, scoped=False)
        self.big0 = self.ps("big0", [128, 1024], F32)
        self.big1 = self.ps("big1", [128, 1024], F32)
        self.s0 = self.ps("s0", [128, 512], F32)
        self.s1 = self.ps("s1", [128, 512], F32)
        self.s2 = self.ps("s2", [128, 512], F32)
        self.pst = self.ps("pst", [128, 1024], BF16)
        P.dma(self.identf[:], d['ident'])
        P.dma(self.identb[:], d['ident'], eng='pool')
        P.dma(self.gains[:], d['gains'])
        P.memset(self.onesb[:], 1.0)
        P.memset(self.epsc[:], EPS)

    def load_x(self, si):
        P = self.P
        self.push()
        xin = [self.sb(f"xin{i}", [128, D], F32) for i in range(2)]
        banks = [self.s0, self.s1]
        for i in range(16):
            xb = xin[i % 2]
            P.dma(xb[:], self.din['x'][si, i * 128:(i + 1) * 128, :])
            for half in range(2):
                pb = banks[half]
                for cc in range(4):
                    c = half * 4 + cc
                    P.transpose(pb[:, cc * 128:(cc + 1) * 128], xb[:, c * 128:(c + 1) * 128], self.identf[:])
                P.copy(self.xT[:, half * 4:(half + 1) * 4, i * 128:(i + 1) * 128],
                       pb[:].rearrange("p (c t) -> p c t", c=4), eng='dve' if half == 0 else 'act')
        self.pop()

    def compute_rstd(self):
        P = self.P
        self.push()
        sqb = self.sb("sqb", [128, 8, 512], BF16)
        banks = [self.s0, self.s1]
        for t in range(NT):
            ts_ = slice(t * 512, (t + 1) * 512)
            P.act(sqb[:], self.xT[:, :, ts_], AF.Square)
            pb = banks[t % 2]
            for c in range(8):
                P.mm(pb[:], self.onesb[:], sqb[:, c, :], start=(c == 0), stop=(c == 7))
            P.act(self.rstd[:, ts_], pb[:], AF.Ln, bias=self.epsc[:, 0:1], scale=1.0 / D)
            P.act(self.rstd[:, ts_], self.rstd[:, ts_], AF.Exp, scale=-0.5)
        self.pop()

    def hn_tile(self, dst, gidx, t0, n=512, flip=0):
        P = self.P
        for c in range(8):
            if c % 2 == 0:
                P.stt(dst[:, c, 0:n], self.xT[:, c, t0:t0 + n], self.gains[:, gidx, c:c + 1], self.rstd[:, t0:t0 + n],
                      ALU.mult, ALU.mult)
            else:
                tmp = self.hntmp[:, (c // 2) % 2, 0:n]
                P.act(tmp, self.xT[:, c, t0:t0 + n], AF.Copy, scale=self.gains[:, gidx, c:c + 1])
                P.tt(dst[:, c, 0:n], tmp, self.rstd[:, t0:t0 + n], ALU.mult, eng='pool')

    def ffn(self, l):
        P, d = self.P, self.din
        self.compute_rstd()
        self.push()
        hnT = self.sb("f_hnT", [128, 8, S], BF16)
        actT = self.sb("f_actT", [128, 4, S], BF16)
        actT2 = self.sb("f_actT2", [128, 4, S], BF16)
        wu = [self.sb(f"f_wu{i}", [128, 8, 256], BF16) for i in range(3)]
        wd = [self.sb(f"f_wd{i}", [128, 4, 1024], BF16) for i in range(2)]
        cpar = self.sb("f_cpar", [128, NPAIR, 2, 4], F32)
        tg = [self.sb(f"f_tg{i}", [128, 684], F32) for i in range(2)]
        tv = [self.sb(f"f_tv{i}", [128, 684], F32) for i in range(2)]
        P.dma(cpar[:], d[f'cpar{l}'])
        for t in range(NT):
            self.hn_tile(hnT[:, :, t * 512:(t + 1) * 512], 2 + l, t * 512)
        dbanks = [self.s0, self.s1, self.s2]
        dcount = [0]
        itc = [0]
        actTs = [actT, actT2]

        def up(gi):
            grp = FGROUPS[gi]
            aT = actTs[gi % 2]
            for jj, j in enumerate(grp):
                wub = wu[j % 3]
                if j == 0:
                    P.dma(wu[0][:], d[f'wup{l}'][0], eng='pool')
                    P.dma(wu[1][:], d[f'wup{l}'][1], eng='pool')
                if j + 2 < NPAIR:
                    P.dma(wu[(j + 2) % 3][:], d[f'wup{l}'][j + 2], eng='pool')
                for (s0_, ncol, olo) in WINS:
                    tgb, tvb = tg[itc[0] % 2], tv[itc[0] % 2]
                    itc[0] += 1
                    for (pb, tb, half, silu) in ((self.big0, tgb, 0, True), (self.big1, tvb, 1, False)):
                        for (c0, cn) in ((0, 512), (512, ncol - 512)):
                            for c in range(8):
                                P.mm(pb[:, c0:c0 + cn], wub[:, c, half * 128:(half + 1) * 128],
                                     hnT[:, c, s0_ + c0:s0_ + c0 + cn], start=(c == 0), stop=(c == 7))
                            yield
                        w0 = cpar[:, j, half, 0:1]; w1 = cpar[:, j, half, 1:2]; w2 = cpar[:, j, half, 2:3]; bb = cpar[:, j, half, 3:4]
                        P.act(tb[:, 0:ncol], pb[:, 0:ncol], AF.Identity, bias=bb, scale=w2)
                        P.stt(tb[:, 1:ncol], pb[:, 0:ncol - 1], w1, tb[:, 1:ncol], ALU.mult, ALU.add)
                        P.stt(tb[:, 2:ncol], pb[:, 0:ncol - 2], w0, tb[:, 2:ncol], ALU.mult, ALU.add)
                        if silu:
                            P.act(tb[:, 0:ncol], tb[:, 0:ncol], AF.Silu)
                    lo = olo - s0_
                    P.tt(aT[:, jj, olo:s0_ + ncol], tgb[:, lo:ncol], tvb[:, lo:ncol], ALU.mult, eng='pool')
                    yield

        def down(gi):
            grp = FGROUPS[gi]
            aT = actTs[gi % 2]
            wdb = wd[gi % 2]
            if gi == 0:
                P.dma(wdb[:, 0:len(grp), :], d[f'wdn{l}'][:, grp[0]:grp[0] + len(grp), :], eng='pool')
            if gi + 1 < len(FGROUPS):
                g2 = FGROUPS[gi + 1]
                P.dma(wd[(gi + 1) % 2][:, 0:len(g2), :], d[f'wdn{l}'][:, g2[0]:g2[0] + len(g2), :], eng='pool')
            for m in range(8):
                for t in range(NT):
                    pb = dbanks[dcount[0] % 3]
                    dcount[0] += 1
                    for jj in range(len(grp)):
                        P.mm(pb[:], wdb[:, jj, m * 128:(m + 1) * 128], aT[:, jj, t * 512:(t + 1) * 512],
                             start=(jj == 0), stop=(jj == len(grp) - 1))
                    xs = self.xT[:, m, t * 512:(t + 1) * 512]
                    P.tt(xs, pb[:], xs, ALU.add)
                    yield

        def interleave(gens):
            gens = [g_ for g_ in gens if g_ is not None]
            while gens:
                for g_ in list(gens):
                    try:
                        next(g_)
                    except StopIteration:
                        gens.remove(g_)

        ng = len(FGROUPS)
        interleave([up(0)])
        for gi in range(ng):
            interleave([down(gi), up(gi + 1) if gi + 1 < ng else None])
        self.pop()

    def final_store(self, si):
        P = self.P
        self.compute_rstd()
        self.push()
        yb = [self.sb(f"o_y{i}", [128, 8, 128], F32) for i in range(2)]
        ob = [self.sb(f"o_o{i}", [128, D], F32) for i in range(2)]
        pbs = [self.big0, self.big1]
        for i in range(16):
            y = yb[i % 2]; o = ob[i % 2]; pb = pbs[i % 2]
            for c in range(8):
                P.stt(y[:, c, :], self.xT[:, c, i * 128:(i + 1) * 128], self.gains[:, 5, c:c + 1],
                      self.rstd[:, i * 128:(i + 1) * 128], ALU.mult, ALU.mult)
            for c in range(8):
                P.transpose(pb[:, c * 128:(c + 1) * 128], y[:, c, :], self.identf[:])
            P.copy(o[:, 0:512], pb[:, 0:512], eng='act')
            P.copy(o[:, 512:1024], pb[:, 512:1024], eng='dve')
            P.dma(self.out[si, i * 128:(i + 1) * 128, :], o[:])
        self.pop()

def _run_seq(self, si):
    self.load_x(si)
    if self.stop == 'load':
        return self.final_store(si)
    self.ffn(0)
    self.final_store(si)
K.run_seq = _run_seq


def _gla(self):
    P, d = self.P, self.din
    self.push()
    sb = self.sb
    oT = sb("g_oT", [128, 8, S], BF16)
    self.compute_rstd()
    self.push()
    hnT = sb("g_hnT", [128, 8, S], BF16)
    wh = [sb(f"g_wh{i}", [128, 8, 768], BF16) for i in range(2)]
    wa = sb("g_wa", [128, 8, 16], BF16)
    waus = [sb(f"g_wau{i}", [16, 128], F32) for i in range(2)]
    bas = [sb(f"g_ba{i}", [128, 128], F32) for i in range(2)]
    gn = sb("g_gn", [128, 1024], F32)
    alT = sb("g_alT", [16, S], F32)
    ltri = sb("g_ltri", [128, 128], F32)
    utri = sb("g_utri", [128, 128], F32)
    onef = sb("g_onef", [128, 128], F32)
    Sf = sb("g_Sf", [128, 256], F32)
    Sb = sb("g_Sb", [128, 256], BF16)
    NB = 2
    qk = [sb(f"g_qk{i}", [128, 256], BF16) for i in range(NB)]
    kh = [sb(f"g_kh{i}", [128, 128], BF16) for i in range(NB)]
    vv = [sb(f"g_vv{i}", [128, 256], BF16) for i in range(NB)]
    sr = [sb(f"g_sr{i}", [128, 256], F32) for i in range(NB)]
    qkT = [sb(f"g_qkT{i}", [128, 256], BF16) for i in range(NB)]
    attm = [sb(f"g_attm{i}", [128, 128], BF16) for i in range(NB)]
    ss = [sb(f"g_ss{i}", [128, 1], F32) for i in range(NB)]
    og = [sb(f"g_og{i}", [128, 256], F32) for i in range(NB)]
    sq = og
    P.dma(wa[:], d['gla_wa'], eng='pool')
    P.dma(waus[0][:], d['gla_wau'][:, 0:128])
    P.dma(bas[0][:], d['gla_ba'][:, 0:128])
    P.dma(gn[:], d['gla_gn'])
    P.dma(ltri[:], d['ltri'])
    P.dma(utri[:], d['utri'])
    P.memset(onef[:], 1.0)
    for t in range(NT):
        self.hn_tile(hnT[:, :, t * 512:(t + 1) * 512], 0, t * 512)
    for t in range(NT):
        for c in range(8):
            P.mm(self.s2[0:16, :], wa[:, c, :], hnT[:, c, t * 512:(t + 1) * 512], start=(c == 0), stop=(c == 7))
        P.copy(alT[:, t * 512:(t + 1) * 512], self.s2[0:16, :])
    ogf = [sb(f"g_ogf{i}", [128, 256], F32) for i in range(NB)]

    gsets = [dict(eb=sb(f"g_eb4{i}", [128, 512], F32), enb=sb(f"g_enb4{i}", [128, 512], F32),
                  erb=sb(f"g_erb4{i}", [128, 512], F32), ebl=sb(f"g_ebl4{i}", [128, 4], F32)) for i in range(2)]
    zb4 = sb("g_zb4", [128, 512], F32)
    gg4 = zb4

    def groupprep(h, G, gs):
        t0g = G * 512
        Z = self.s2
        for cc in range(4):
            P.mm(Z[:, cc * 128:(cc + 1) * 128], alT[0:16, t0g + cc * 128:t0g + (cc + 1) * 128], waus[h % 2][0:16, :])
        yield
        for cc in range(4):
            P.tt(zb4[:, cc * 128:(cc + 1) * 128], Z[:, cc * 128:(cc + 1) * 128], bas[h % 2][:, :], ALU.add)
        yield
        P.act(zb4[:], zb4[:], AF.Exp, scale=-1.0)
        yield
        P.act(zb4[:], zb4[:], AF.Ln, bias=onef[:, 0:1])
        yield
        P.ts(gg4[:], zb4[:], -1.0 / 16.0, None, ALU.mult)
        yield
        P.mm(Z[:, :], ltri[:], gg4[:])
        yield
        P.act(gs['eb'][:], Z[:, :], AF.Exp)
        P.act(gs['enb'][:], Z[:, :], AF.Exp, scale=-1.0)
        yield
        P.mm(Z[:, :], utri[:], gg4[:])
        yield
        P.act(gs['erb'][:], Z[:, :], AF.Exp)
        yield
        for cc in range(4):
            P.mm(Z[:, cc * 128:(cc + 1) * 128], gg4[:, cc * 128:(cc + 1) * 128], onef[:])
        yield
        P.act(gs['ebl'][:], Z[:, 0:512:128], AF.Exp)
        yield

    def phaseA(h, i, b_):
        whb = wh[h % 2]
        t0 = i * 128
        gs = gsets[(h * 4 + i // 4) % 2]
        cs = slice((i % 4) * 128, (i % 4 + 1) * 128)
        A = self.s0
        Bp = self.s1[:, 0:256]
        W = self.big0
        for c in range(8):
            P.mm(A[:], hnT[:, c, t0:t0 + 128], whb[:, c, 0:512], start=(c == 0), stop=(c == 7))
        for c in range(8):
            P.mm(Bp, hnT[:, c, t0:t0 + 128], whb[:, c, 512:768], start=(c == 0), stop=(c == 7))
        yield
        P.copy(vv[b_][:], A[:, 256:512], eng='act')
        P.act(sr[b_][:], Bp, AF.Exp, scale=-1.0)
        P.stt(qk[b_][:, 0:128], A[:, 0:128], float(128 ** -0.5), gs['eb'][:, cs], ALU.mult, ALU.mult)
        P.tt(qk[b_][:, 128:256], A[:, 128:256], gs['enb'][:, cs], ALU.mult)
        yield
        P.tt(kh[b_][:], A[:, 128:256], gs['erb'][:, cs], ALU.mult)
        P.ts(sr[b_][:], sr[b_][:], 1.0, None, ALU.add)
        P.transpose(self.pst[:, 0:128], qk[b_][:, 0:128], self.identb[:])
        P.transpose(self.pst[:, 128:256], qk[b_][:, 128:256], self.identb[:])
        yield
        P.copy(qkT[b_][:], self.pst[:, 0:256])
        P.op('dve', lambda e, a=sr[b_][:]: e.reciprocal(a, a), reads=[sr[b_][:]], writes=[sr[b_][:]])
        yield
        P.mm(W[:, 0:128], qkT[b_][:, 128:256], qkT[b_][:, 0:128])
        P.tt(sr[b_][:], Bp, sr[b_][:], ALU.mult)
        yield
        P.tt(attm[b_][:], W[:, 0:128], ltri[:], ALU.mult)
        yield

    def phaseB(h, i, b_):
        t0 = i * 128
        Wo = self.big0[:, 512:768]
        Ws = self.big1[:, 0:256]
        Wt = self.big1[:, 512:768]
        P.mm(Wo, qkT[b_][:, 0:128], Sb[:], start=True, stop=False)
        P.mm(Wo, attm[b_][:], vv[b_][:], start=False, stop=True)
        P.mm(Ws, kh[b_][:], vv[b_][:])
        yield
        P.stt(Sf[:], Sf[:], gsets[(h * 4 + i // 4) % 2]['ebl'][:, (i % 4):(i % 4) + 1], Ws, ALU.mult, ALU.add)
        P.act(sq[b_][:], Wo, AF.Square)
        yield
        P.copy(Sb[:], Sf[:], eng='pool')
        P.op('dve', lambda e, o_=ss[b_][:], i_=sq[b_][:]: e.reduce_sum(o_, i_, axis=AX.X), reads=[sq[b_][:]], writes=[ss[b_][:]])
        yield
        P.act(ss[b_][:], ss[b_][:], AF.Ln, bias=self.epsc[:, 0:1], scale=1.0 / 256)
        yield
        P.act(ss[b_][:], ss[b_][:], AF.Exp, scale=-0.5)
        yield
        P.stt(og[b_][:], Wo, ss[b_][:, 0:1], gn[:, h * 256:(h + 1) * 256], ALU.mult, ALU.mult)
        yield
        P.tt(ogf[b_][:], og[b_][:], sr[b_][:], ALU.mult, eng='pool')
        yield
        for j in range(2):
            P.transpose(Wt[:, j * 128:(j + 1) * 128], ogf[b_][:, j * 128:(j + 1) * 128], self.identf[:])
        yield
        P.copy(oT[:, 2 * h:2 * h + 2, t0:t0 + 128], Wt.rearrange("p (j t) -> p j t", j=2), eng='act')
        yield

    def interleave(gens):
        gens = [g_ for g_ in gens if g_ is not None]
        while gens:
            for g_ in list(gens):
                try:
                    next(g_)
                except StopIteration:
                    gens.remove(g_)

    P.dma(wh[0][:], d['gla_wh'][0], eng='pool')
    seq = [(h, i) for h in range(4) for i in range(16)]
    interleave([groupprep(0, 0, gsets[0])])
    interleave([phaseA(0, 0, 0)])
    for n_, (h, i) in enumerate(seq):
        if i == 0:
            P.memset(Sf[:], 0.0, eng='dve')
            P.memset(Sb[:], 0.0, eng='dve')
            if h + 1 < 4:
                P.dma(wh[(h + 1) % 2][:], d['gla_wh'][h + 1], eng='pool')
                P.dma(waus[(h + 1) % 2][:], d['gla_wau'][:, (h + 1) * 128:(h + 2) * 128])
                P.dma(bas[(h + 1) % 2][:], d['gla_ba'][:, (h + 1) * 128:(h + 2) * 128])
        nxt = seq[n_ + 1] if n_ + 1 < len(seq) else None
        prep = None
        if i % 4 == 0:
            gidx = h * 4 + i // 4 + 1
            if gidx < 16:
                prep = groupprep(gidx // 4, gidx % 4, gsets[gidx % 2])
        interleave([phaseB(h, i, n_ % NB), phaseA(nxt[0], nxt[1], (n_ + 1) % NB) if nxt else None, prep])
    self.pop()
    self.push()
    wo0 = sb("g_wo0", [128, 8, 512], BF16)
    wo1 = sb("g_wo1", [128, 8, 512], BF16)
    P.dma(wo0[:], d['gla_wo'][:, :, 0:512], eng='pool')
    P.dma(wo1[:], d['gla_wo'][:, :, 512:1024], eng='pool')
    banks = [self.s0, self.s1, self.s2]
    n = 0
    for m in range(8):
        wb = wo0 if m < 4 else wo1
        mm_ = m % 4
        for t in range(NT):
            pb = banks[n % 3]
            n += 1
            for c in range(8):
                P.mm(pb[:], wb[:, c, mm_ * 128:(mm_ + 1) * 128], oT[:, c, t * 512:(t + 1) * 512], start=(c == 0), stop=(c == 7))
            xs = self.xT[:, m, t * 512:(t + 1) * 512]
            P.tt(xs, pb[:], xs, ALU.add)
    self.pop()
    self.pop()


K.gla = _gla


def _run_seq(self, si):
    self.load_x(si)
    st = self.stop
    if st == 'load':
        return self.final_store(si)
    if st == 'ffn0':
        self.ffn(0)
        return self.final_store(si)
    self.gla()
    if st == 'gla':
        return self.final_store(si)
    self.final_store(si)


K.run_seq = _run_seq


BIG = 30000.0
SCALE = 0.125


def _nsa(self):
    P, d = self.P, self.din
    sb = self.sb
    self.compute_rstd()
    self.push()
    oT = sb("n_oT", [128, 8, S], BF16)
    LK = sb("n_LK", [96, S], BF16)
    kwT = sb("n_kwT", [64, S], BF16)
    V1 = sb("n_V1", [128, 16, 2, 65], BF16)
    kcT = sb("n_kcT", [64, 128], BF16)
    vc = sb("n_vc", [128, 64], BF16)
    hnb = sb("n_hnb", [128, 8, 512], BF16)
    rc = sb("n_rc", [64, 512], F32)
    rs = sb("n_rs", [64, 512], F32)
    r1 = sb("n_r1", [64, 512], F32)
    r2 = sb("n_r2", [64, 512], F32)
    P.dma(LK[64:96, :], d['expand'], eng='pool')
    sc_banks = [self.s0, self.s1, self.s2]
    scn = [0]

    def scbank():
        b = sc_banks[scn[0] % 3]
        scn[0] += 1
        return b

    def rope_to(dst, pa, pb_):
        P.tt(r1[:], pa, rc[:], ALU.mult)
        P.tt(r2[:], pb_, rs[:], ALU.mult)
        P.tt(dst, r1[:], r2[:], ALU.add, eng='pool')

    for hk in range(4):
        self.push()
        wkv = sb("n_wkv", [128, 8, 512], BF16)
        rawT = sb("n_rawT", [64, 2, S], BF16)
        w1bs = [sb(f"n_w1b{i}", [64, 32, 256], BF16) for i in range(2)]
        w2bs = [sb(f"n_w2b{i}", [128, 2, 64], BF16) for i in range(2)]
        pefs = [sb(f"n_pef{i}", [64, 32], F32) for i in range(2)]
        hnb2 = sb("n_hnb2", [128, 8, 512], BF16)
        pe2 = sb("n_pe2", [64, 32, 2], BF16)
        cb = sb("n_cb", [128, 2], F32)
        hx = sb("n_hx", [128, 127], F32)
        hx2 = sb("n_hx2", [128, 127], F32)
        hidT = sb("n_hidT", [128, 2, 127], BF16)
        P.dma(wkv[:], d['nsa_wkv'][hk], eng='pool')
        for kind in range(2):
            P.dma(w1bs[kind][:], d['cmp_w1'][kind], eng='pool')
            P.dma(w2bs[kind][:], d['cmp_w2'][kind], eng='pool')
            P.dma(pefs[kind][:], d['cmp_peT'][kind])
        P.memset(V1[:], 1.0, eng='dve')
        hnb_q = hnb
        for tt in range(NT):
            t0 = tt * 512
            hnb = hnb_q if tt % 2 == 0 else hnb2
            self.hn_tile(hnb, 4, t0)
            P.dma(rc[:], d['ropec'][:, t0:t0 + 512])
            P.dma(rs[:], d['ropes'][:, t0:t0 + 512])
            pbs = []
            for u in range(6):
                pb = scbank() if u < 2 else [self.big0[:, 0:512], self.big0[:, 512:1024], self.big1[:, 0:512], self.big1[:, 512:1024]][u - 2]
                for c in range(8):
                    P.mm(pb[0:64, 0:512], wkv[:, c, u * 64:(u + 1) * 64], hnb[:, c, :], start=(c == 0), stop=(c == 7))
                pbs.append(pb)
                if u < 2:
                    P.copy(rawT[:, u, t0:t0 + 512], pb[0:64, 0:512], eng='act')
            rope_to(LK[0:64, t0:t0 + 512], pbs[2][0:64, 0:512], pbs[3][0:64, 0:512])
            rope_to(kwT[0:64, t0:t0 + 512], pbs[4][0:64, 0:512], pbs[5][0:64, 0:512])
            for sub in range(4):
                pb = scbank()
                for c in range(8):
                    P.mm(pb[:, 0:128], hnb[:, c, sub * 128:(sub + 1) * 128], wkv[:, c, 384:512], start=(c == 0), stop=(c == 7))
                P.copy(V1[:, tt * 4 + sub, :, 0:64], pb[:, 0:128].rearrange("p (b e) -> p b e", b=2), eng='act')
        hnb = hnb_q
        for kind in range(2):
            w1b = w1bs[kind]; w2b = w2bs[kind]; pef = pefs[kind]
            P.copy(pe2[:, :, 0], pef[:])
            P.copy(pe2[:, :, 1], pef[:])
            for ht in range(2):
                pb = scbank()
                for j in range(32):
                    P.mm(pb[:, 0:2], w1b[:, j, ht * 128:(ht + 1) * 128], pe2[:, j, :], start=(j == 0), stop=(j == 31))
                P.copy(cb[:, ht:ht + 1], pb[:, 0:1])
                pb = scbank()
                for j in range(32):
                    P.mm(pb[:, 0:127], w1b[:, j, ht * 128:(ht + 1) * 128], rawT[0:64, kind, j:j + 2017:16],
                         start=(j == 0), stop=(j == 31))
                P.act(hx[:], pb[:, 0:127], AF.Identity, bias=cb[:, ht:ht + 1])
                P.tt(hx2[:], hx[:], hx[:], ALU.mult)
                P.ts(hx2[:], hx2[:], 0.044715, 1.0, ALU.mult, ALU.add)
                P.tt(hx2[:], hx2[:], hx[:], ALU.mult)
                P.act(hx2[:], hx2[:], AF.Exp, scale=-1.5957691216057308)
                P.ts(hx2[:], hx2[:], 1.0, None, ALU.add)
                P.op('dve', lambda e, a=hx2[:]: e.reciprocal(a, a), reads=[hx2[:]], writes=[hx2[:]])
                P.tt(hidT[:, ht, :], hx[:], hx2[:], ALU.mult)
            pb = scbank()
            if kind == 0:
                for ht in range(2):
                    P.mm(pb[0:64, 0:127], w2b[:, ht, :], hidT[:, ht, :], start=(ht == 0), stop=(ht == 1))
                P.copy(kcT[:, 0:127], pb[0:64, 0:127])
            else:
                for ht in range(2):
                    P.mm(pb[0:127, 0:64], hidT[:, ht, :], w2b[:, ht, :], start=(ht == 0), stop=(ht == 1))
                P.copy(vc[0:127, :], pb[0:127, 0:64])
        self.pop()
        self.push()
        wq = sb("n_wq", [128, 8, 512], BF16)
        wg = sb("n_wg", [128, 8, 12], BF16)
        RQs = [sb(f"n_RQ{i}", [96, 4, 512], BF16) for i in range(2)]
        qnat = sb("n_qnat", [64, 4, 512], BF16)
        band = sb("n_band", [128, 8, 512], BF16)
        cmask = sb("n_cmask", [128, 512], BF16)
        selA = sb("n_selA", [128, 4, 32], F32)
        selB = sb("n_selB", [128, 4, 32], F32)
        wov = sb("n_wov", [128, 32], BF16)
        NPT = 8
        pT = [sb(f"n_pT{i}", [128, 512], BF16) for i in range(NPT)]
        cpt = [sb(f"n_cpt{i}", [128, 512], BF16) for i in range(2)]
        dens = [sb(f"n_den{i}", [128, 512], F32) for i in range(2)]
        pns = [sb(f"n_pn{i}", [128, 512], BF16) for i in range(2)]
        gts = [sb(f"n_gt{i}", [128, 4, 12], F32) for i in range(2)]
        otoks = [sb(f"n_otok{i}", [128, 4, 256], F32) for i in range(2)]
        otb = sb("n_otb", [128, 4, 256], BF16)
        impa = sb("n_impa", [128, 4, 32], F32)
        imp2 = sb("n_imp2", [128, 32], F32)
        imp3 = sb("n_imp3", [128, 32], F32)
        m8 = sb("n_m8", [128, 16], F32)
        msel = sb("n_msel", [128, 32], F32)
        negm = sb("n_negm", [128, 4, 96], BF16)
        rden = sb("n_rden", [128, 4], F32)
        fac = sb("n_fac", [128, 4], F32)
        rcs = sb("n_rcs", [128, 512], F32)
        r12s = [sb(f"n_r12{i}", [128, 512], F32) for i in range(2)]
        r2ss = [sb(f"n_r2s{i}", [64, 512], F32) for i in range(2)]
        P.dma(wq[:], d['nsa_wq'][hk], eng='pool')
        P.dma(wg[:], d['nsa_wg'][hk], eng='pool')
        P.dma(band[:], d['band'], eng='pool')
        P.dma(wov[0:127, :], d['wov'], eng='pool')
        P.memset(negm[:], 0.0, eng='dve')
        pti = [0]

        def next_pT():
            b = pT[pti[0] % NPT]
            pti[0] += 1
            return b

        B1 = [self.big1[:, 0:512], self.big1[:, 512:1024]]

        def prologue(qt):
            q0 = qt * 512
            qb = q0 // 128
            RQ = RQs[qt % 2]; gt = gts[qt % 2]; otok = otoks[qt % 2]
            self.hn_tile(hnb, 1, q0)
            P.dma(rcs[0:64, :], d['ropec'][:, q0:q0 + 512])
            P.dma(rcs[64:128, :], d['ropes'][:, q0:q0 + 512])
            P.dma(cmask[0:127, :], d['cmpmask'][:, q0:q0 + 512], eng='pool')
            P.dma(selA[:], d['selA'][:, qb:qb + 4, :])
            P.dma(selB[:], d['selB'][:, qb:qb + 4, :])
            yield
            for sub in range(4):
                pb = B1[sub % 2]
                for c in range(8):
                    P.mm(pb[:, 0:12], hnb[:, c, sub * 128:(sub + 1) * 128], wg[:, c, :], start=(c == 0), stop=(c == 7))
                P.act(gt[:, sub, :], pb[:, 0:12], AF.Exp, scale=-1.0)
                yield
            P.ts(gt[:], gt[:], 1.0, None, ALU.add)
            P.op('dve', lambda e, a=gt[:]: e.reciprocal(a, a), reads=[gt[:]], writes=[gt[:]])
            P.memset(impa[:], 0.0, eng='dve')

            def gchain(g):
                pa = B1[g % 2]
                r12 = r12s[g % 2]; r2s = r2ss[g % 2]
                for c in range(8):
                    P.mm(pa[:, :], wq[:, c, g * 128:(g + 1) * 128], hnb[:, c, :], start=(c == 0), stop=(c == 7))
                yield
                P.copy(qnat[:, g, :], pa[0:64, :], eng='act')
                P.tt(r12[:], pa[:, :], rcs[:], ALU.mult)
                yield
                P.copy(r2s[:], r12[64:128, :], eng='pool')
                yield
                P.tt(RQ[0:64, g, :], r12[0:64, :], r2s[:], ALU.add, eng='pool')
                yield
                pb = B1[g % 2]
                pt = cpt[g % 2]; den = dens[g % 2]; pn = pns[g % 2]
                P.mm(pb[0:127, :], kcT[:, 0:127], qnat[:, g, :])
                yield
                P.act(pt[0:127, :], pb[0:127, :], AF.Exp, scale=SCALE)
                yield
                P.tt(pt[0:127, :], pt[0:127, :], cmask[0:127, :], ALU.mult, eng='pool')
                yield
                P.mm(pb[:, :], self.onesb[0:127, :], pt[0:127, :])
                yield
                P.ts(den[:], pb[:, :], 1e-30, None, ALU.add)
                yield
                P.op('dve', lambda e, a=den[:]: e.reciprocal(a, a), reads=[den[:]], writes=[den[:]])
                yield
                P.tt(pn[0:127, :], pt[0:127, :], den[0:127, :], ALU.mult, eng='pool')
                yield
                for sub in range(4):
                    P.mm(pb[:, sub * 96:sub * 96 + 64], pn[0:127, sub * 128:(sub + 1) * 128], vc[0:127, :])
                    P.mm(pb[:, sub * 96 + 64:sub * 96 + 96], pn[0:127, sub * 128:(sub + 1) * 128], wov[0:127, :])
                yield
                pv = pb[:, 0:384].rearrange("p (s c) -> p s c", c=96)
                for sub in range(4):
                    P.ts(otok[:, sub, g * 64:(g + 1) * 64], pv[:, sub, 0:64], gt[:, sub, g:g + 1], None, ALU.mult)
                P.tt(impa[:], pv[:, :, 64:96], impa[:], ALU.add)
                yield

            for gp in range(2):
                ga, gb = gchain(2 * gp), gchain(2 * gp + 1)
                alive = [ga, gb]
                while alive:
                    for g_ in list(alive):
                        try:
                            next(g_)
                        except StopIteration:
                            alive.remove(g_)
                    yield
            for sub in range(4):
                P.tt(imp2[:], impa[:, sub, :], selA[:, sub, :], ALU.mult)
                P.tt(imp2[:], imp2[:], selB[:, sub, :], ALU.add)
                yield
                P.op('dve', lambda e: e.max(m8[:, 0:8], imp2[:]), reads=[imp2[:]], writes=[m8[:, 0:8]])
                P.op('dve', lambda e: e.match_replace(imp3[:], m8[:, 0:8], imp2[:], -1e30), reads=[m8[:, 0:8], imp2[:]], writes=[imp3[:]])
                yield
                P.op('dve', lambda e: e.max(m8[:, 8:16], imp3[:]), reads=[imp3[:]], writes=[m8[:, 8:16]])
                P.ts(msel[:], imp2[:], m8[:, 15:16], None, ALU.is_ge)
                yield
                P.ts(negm[:, sub, 64:96], msel[:], 1.0, BIG, ALU.subtract, ALU.mult)
                yield
                P.transpose(self.pst[0:96, 512 + sub * 128:512 + (sub + 1) * 128], negm[:, sub, :], self.identb[:])
                yield
            for g in range(4):
                P.copy(RQ[64:96, g, :], self.pst[64:96, 512:1024], eng='dve')
            yield

        def mainloop(qt):
            q0 = qt * 512
            qb = q0 // 128
            RQ = RQs[qt % 2]; gt = gts[qt % 2]; otok = otoks[qt % 2]
            tiles = []
            for br in range(2):
                for g in range(4):
                    kts = list(range(0, qb + 4)) if br == 0 else list(range(max(0, qb - 4), qb + 4))
                    for ii, kt in enumerate(kts):
                        tiles.append((br, g, kt, ii == 0, ii == len(kts) - 1))
            LOOK = 4
            pts = {}
            main_banks = [self.s0, self.s1, self.s2]

            def st_score(i):
                br, g, kt, first, last = tiles[i]
                r = kt - qb + 4
                pb = main_banks[i % len(main_banks)]
                masked = (br == 1 or r >= 4)
                if br == 0:
                    P.mm(pb[:, 0:512], LK[0:96, kt * 128:(kt + 1) * 128], RQ[0:96, g, :])
                else:
                    P.mm(pb[:, 0:512], kwT[0:64, kt * 128:(kt + 1) * 128], RQ[0:64, g, :])
                pt = next_pT()
                pts[i] = pt
                P.act(pt[:], pb[:, 0:512], AF.Exp, scale=SCALE)
                if masked:
                    P.tt(pt[:], pt[:], band[:, r, :], ALU.mult)

            def st_pv(i):
                br, g, kt, first, last = tiles[i]
                r = kt - qb + 4
                O = self.big0[:, 0:260] if (g % 2 == 0) else self.big0[:, 512:772]
                pt = pts.pop(i)
                if first:
                    P.memset(O, 0.0, eng='dve')
                for sub in range(4):
                    if r - sub > 4 or (br == 1 and r - sub < 0):
                        continue
                    P.mm(O[:, sub * 65:(sub + 1) * 65], pt[:, sub * 128:(sub + 1) * 128], V1[:, kt, br, :],
                         start=False, stop=False, skip=True)
                if last:
                    Ov = O.rearrange("p (s c) -> p s c", c=65)
                    P.op('dve', lambda e, o_=rden[:], i_=Ov[:, :, 64]: e.reciprocal(o_, i_), reads=[O], writes=[rden[:]])
                    P.tt(fac[:], rden[:], gt[:, :, (br + 1) * 4 + g], ALU.mult)
                    for sub in range(4):
                        os_ = otok[:, sub, g * 64:(g + 1) * 64]
                        P.stt(os_, Ov[:, sub, 0:64], fac[:, sub:sub + 1], os_, ALU.mult, ALU.add)

            for i in range(len(tiles) + LOOK):
                if i < len(tiles):
                    st_score(i)
                if i - LOOK >= 0:
                    st_pv(i - LOOK)
                yield
            P.copy(otb[:], otok[:], eng='pool')
            for sub in range(4):
                for j in range(2):
                    P.transpose(self.pst[:, j * 128:(j + 1) * 128], otb[:, sub, j * 128:(j + 1) * 128], self.identb[:])
                P.copy(oT[:, 2 * hk:2 * hk + 2, q0 + sub * 128:q0 + (sub + 1) * 128],
                       self.pst[:, 0:256].rearrange("p (j t) -> p j t", j=2), eng='act')
            yield

        def interleave(gens):
            gens = [g_ for g_ in gens if g_ is not None]
            while gens:
                for g_ in list(gens):
                    try:
                        next(g_)
                    except StopIteration:
                        gens.remove(g_)

        order = [3, 2, 1, 0]
        interleave([prologue(order[0])])
        for oi, qt in enumerate(order):
            interleave([mainloop(qt), prologue(order[oi + 1]) if oi + 1 < NT else None])
        self.pop()
    self.push()
    wo0 = sb("n_wo0", [128, 8, 512], BF16)
    wo1 = sb("n_wo1", [128, 8, 512], BF16)
    P.dma(wo0[:], d['nsa_wo2'][:, :, 0:512], eng='pool')
    P.dma(wo1[:], d['nsa_wo2'][:, :, 512:1024], eng='pool')
    n = 0
    for m in range(8):
        wb = wo0 if m < 4 else wo1
        mm_ = m % 4
        for t in range(NT):
            pb = sc_banks[n % 3]
            n += 1
            for c in range(8):
                P.mm(pb[:], wb[:, c, mm_ * 128:(mm_ + 1) * 128], oT[:, c, t * 512:(t + 1) * 512], start=(c == 0), stop=(c == 7))
            xs = self.xT[:, m, t * 512:(t + 1) * 512]
            P.tt(xs, pb[:], xs, ALU.add)
    self.pop()
    self.pop()


K.nsa = _nsa


def _run_seq(self, si):
    self.load_x(si)
    st = self.stop
    if st == 'load':
        return self.final_store(si)
    if st == 'ffn0':
        self.ffn(0)
        return self.final_store(si)
    if st == 'nsa':
        self.nsa()
        return self.final_store(si)
    self.gla()
    if st == 'gla':
        return self.final_store(si)
    self.ffn(0)
    self.nsa()
    self.ffn(1)
    self.final_store(si)


K.run_seq = _run_seq


N_CORES = 8


def build_program(nseq, shapes, stop=None):
    from contextlib import ExitStack
    nc = bass.Bass("TRN2", target_bir_lowering=False)
    with ExitStack() as stack:
        P = Prog(nc)
        k = K(nc, P, stack, nseq, shapes, stop)
        k.setup()
        for si in range(nseq):
            k.run_seq(si)
        P.emit(stack)
    return nc, P


def run(inputs, n_cores=N_CORES, stop=None, trace=False):
    x = np.ascontiguousarray(inputs['x'], dtype=np.float32)
    B = x.shape[0]
    nseq = B // n_cores
    shared = host_prep(inputs)
    shared.update(host_consts())
    shapes = {k: v.shape for k, v in shared.items()}
    shapes['x'] = (nseq, S, D)
    nc, P = build_program(nseq, shapes, stop)
    in_maps = []
    for c in range(n_cores):
        m = dict(shared)
        m['x'] = x[c * nseq:(c + 1) * nseq]
        in_maps.append(m)
    res = run_bass_kernel_spmd(nc, in_maps, core_ids=list(range(n_cores)), trace=trace)
    out = np.concatenate([r['out'] for r in res.results], axis=0)
    return out, res, P


def kernel(**inputs):
    out, _, _ = run(inputs)
    return out.astype(np.float32)
```

```python
import numpy as np
import concourse.bass as bass
import concourse.mybir as mybir
from concourse.bass_utils import run_bass_kernel_spmd

F32 = mybir.dt.float32
BF16 = mybir.dt.bfloat16
AF = mybir.ActivationFunctionType
ALU = mybir.AluOpType
AX = mybir.AxisListType

SEM_CH = 16000
N_DMA_SEMS = 24


def _region(ap):
    t = ap.tensor
    cls = type(t).__name__
    if 'DRam' in cls or 'Dram' in cls or 'DRAM' in cls:
        return None
    pairs = ap.ap
    shape = t.shape
    pstride = 1
    for s in shape[1:]:
        pstride *= s
    off = int(ap.offset)
    p0 = off // pstride
    f0 = off % pstride
    if pairs[0][0] == pstride or pairs[0][1] == 1:
        pc = pairs[0][1]
        rest = pairs[1:]
    else:
        pc = 1
        rest = pairs
    ext = 0
    for st, cnt in rest:
        ext += (cnt - 1) * abs(st)
    f1 = f0 + ext + 1
    if 'PSum' in cls or 'Psum' in cls or 'PSUM' in cls:
        be = 2048 // (2 if 'bfloat16' in str(ap.dtype) or 'float16' in str(ap.dtype) else 4)
        return (t.name, 0, 128, (f0 // be) * be, ((f1 + be - 1) // be) * be, True)
    return (t.name, p0, p0 + pc, f0, f1, False)


def _overlap(a, b):
    return a[1] < b[2] and b[1] < a[2] and a[3] < b[4] and b[3] < a[4]


def _contains(a, b):
    return a[1] <= b[1] and b[2] <= a[2] and a[3] <= b[3] and b[4] <= a[4]


class Prog:
    ENGS = ('pe', 'act', 'dve', 'pool', 'sp')

    def __init__(self, nc):
        self.nc = nc
        self.ops = []
        self.hist = {}
        self.bar_pending = set()
        self.bar_deps = set()
        self.bar_deps_prev = set()
        self.bar_start = 0

    def barrier(self):
        last = {}
        dmas = set()
        for o in self.ops[self.bar_start:]:
            if o['dma']:
                dmas.add(o['idx'])
            else:
                last[o['eng']] = o['idx']
        self.bar_deps = set(last.values()) | dmas | set(self.bar_deps_prev)
        self.bar_deps_prev = set(last.values())
        self.bar_pending = set(self.ENGS)
        self.bar_start = len(self.ops)
        self.hist = {}

    def op(self, eng, fn, reads=(), writes=(), dma=False):
        idx = len(self.ops)
        deps = set()
        if eng in self.bar_pending:
            deps |= self.bar_deps
            self.bar_pending.discard(eng)
        rr = [r for r in (_region(a) for a in reads) if r is not None]
        ww = [r for r in (_region(a) for a in writes) if r is not None]
        ww = ww + [r for r in rr if r[5]]
        rr = [r for r in rr if not r[5]]
        for r in rr:
            for rec in self.hist.get(r[0], ()):
                if rec[6] and _overlap(r, rec):
                    deps.add(rec[5])
        for w in ww:
            for rec in self.hist.get(w[0], ()):
                if _overlap(w, rec):
                    deps.add(rec[5])
        deps.discard(idx)
        for w in ww:
            lst = self.hist.setdefault(w[0], [])
            lst[:] = [rec for rec in lst if not _contains(w, rec)]
            lst.append((w[0], w[1], w[2], w[3], w[4], idx, True, eng, dma))
        for r in rr:
            lst = self.hist.setdefault(r[0], [])
            if not dma:
                lst[:] = [rec for rec in lst
                          if not ((not rec[6]) and rec[7] == eng and not rec[8] and _contains(r, rec))]
            lst.append((r[0], r[1], r[2], r[3], r[4], idx, False, eng, dma))
        self.ops.append(dict(eng=eng, fn=fn, deps=deps, dma=dma, idx=idx))
        return idx

    def emit(self, stack):
        nc = self.nc
        ops = self.ops
        signal = [False] * len(ops)
        for o in ops:
            for d in o['deps']:
                od = ops[d]
                if od['eng'] == 'pe' and o['eng'] == 'pe' and not od['dma'] and not o['dma']:
                    continue
                signal[d] = True
        eng_count = {e: 0 for e in self.ENGS}
        eng_sems = {e: [] for e in self.ENGS}
        dma_sems = [stack.enter_context(nc.semaphore(f"dsem{i}")) for i in range(N_DMA_SEMS)]
        dma_tot = [0] * N_DMA_SEMS
        dma_last = [None] * N_DMA_SEMS
        n_sw = 4
        n_hw = N_DMA_SEMS - n_sw
        dma_rr = {'sw': 0, 'hw': 0}
        token = [None] * len(ops)
        extra_dep = [None] * len(ops)
        for o in ops:
            i = o['idx']
            if o['dma']:
                kind = 'sw' if o['eng'] == 'pool' else 'hw'
                k = (dma_rr[kind] % n_sw) + n_hw if kind == 'sw' else (dma_rr[kind] % n_hw)
                dma_rr[kind] += 1
                extra_dep[i] = dma_last[k]
                dma_tot[k] += 16
                token[i] = (dma_sems[k], dma_tot[k], 16)
                dma_last[k] = i
            elif signal[i]:
                e = o['eng']
                c = eng_count[e]
                ep = c // SEM_CH
                if ep >= len(eng_sems[e]):
                    eng_sems[e].append(stack.enter_context(nc.semaphore(f"s_{e}{ep}")))
                token[i] = (eng_sems[e][ep], c % SEM_CH + 1, 1)
                eng_count[e] = c + 1
        self.n_signal = dict(eng_count)
        block = stack.enter_context(nc.Block())

        def run_engine(e, eng_obj):
            waited = {}
            my_dma = set()
            for o in ops:
                if o['eng'] != e:
                    continue
                i = o['idx']
                deps = set(o['deps'])
                if extra_dep[i] is not None:
                    deps.add(extra_dep[i])
                need = {}
                for d in deps:
                    od = ops[d]
                    if od['eng'] == 'pe' and e == 'pe' and not od['dma'] and not o['dma']:
                        continue
                    sem, val, _ = token[d]
                    key = id(sem)
                    if waited.get(key, 0) >= val:
                        continue
                    if key not in need or need[key][1] < val:
                        need[key] = (sem, val)
                for key, (sem, val) in need.items():
                    eng_obj.wait_ge(sem, val)
                    waited[key] = val
                ins = o['fn'](eng_obj)
                if token[i] is not None:
                    sem, val, inc = token[i]
                    ins.then_inc(sem, inc)
                    if o['dma']:
                        my_dma.add(i)
            fin = {}
            for i in my_dma:
                sem, val, _ = token[i]
                key = id(sem)
                if key not in fin or fin[key][1] < val:
                    fin[key] = (sem, val)
            for key, (sem, val) in fin.items():
                if waited.get(key, 0) < val:
                    eng_obj.wait_ge(sem, val)

        @block.tensor
        def _(pe):
            run_engine('pe', pe)

        @block.scalar
        def _(act):
            run_engine('act', act)

        @block.vector
        def _(dve):
            run_engine('dve', dve)

        @block.gpsimd
        def _(pool):
            run_engine('pool', pool)

        @block.sync
        def _(sp):
            run_engine('sp', sp)

    def dma(self, out, in_, eng='sp'):
        return self.op(eng, lambda e: e.dma_start(out=out, in_=in_), reads=[in_], writes=[out], dma=True)

    def mm(self, out, lhsT, rhs, start=True, stop=True, skip=False):
        rd = [lhsT, rhs] + ([] if start else [out])
        if skip:
            return self.op('pe', lambda e: e.matmul(out, lhsT, rhs, start=start, stop=stop, skip_group_check=True), reads=rd, writes=[out])
        return self.op('pe', lambda e: e.matmul(out, lhsT, rhs, start=start, stop=stop), reads=rd, writes=[out])

    def transpose(self, out, in_, ident):
        return self.op('pe', lambda e: e.transpose(out, in_, ident), reads=[in_, ident], writes=[out])

    def act(self, out, in_, func, bias=None, scale=None, accum_out=None, eng='act'):
        kw = {}
        rd = [in_]
        wr = [out]
        if bias is not None:
            kw['bias'] = bias
            if not isinstance(bias, (int, float)):
                rd.append(bias)
        if scale is not None:
            kw['scale'] = scale
            if not isinstance(scale, (int, float)):
                rd.append(scale)
        if accum_out is not None:
            kw['accum_out'] = accum_out
            wr.append(accum_out)
        return self.op(eng, lambda e: e.activation(out, in_, func, **kw), reads=rd, writes=wr)

    def tt(self, out, in0, in1, op, eng='dve'):
        return self.op(eng, lambda e: e.tensor_tensor(out, in0, in1, op), reads=[in0, in1], writes=[out])

    def ts(self, out, in0, s1, s2, op0, op1=None, eng='dve', accum_out=None):
        rd = [in0] + [s for s in (s1, s2) if s is not None and not isinstance(s, (int, float))]
        wr = [out] + ([accum_out] if accum_out is not None else [])
        kw = {}
        if op1 is not None:
            kw['op1'] = op1
        if accum_out is not None:
            kw['accum_out'] = accum_out
        return self.op(eng, lambda e: e.tensor_scalar(out, in0, s1, s2, op0, **kw), reads=rd, writes=wr)

    def stt(self, out, in0, scalar, in1, op0, op1, eng='dve'):
        rd = [in0, in1] + ([] if isinstance(scalar, (int, float)) else [scalar])
        return self.op(eng, lambda e: e.scalar_tensor_tensor(out, in0, scalar, in1, op0, op1), reads=rd, writes=[out])

    def copy(self, out, in_, eng='dve'):
        if eng == 'act':
            return self.op(eng, lambda e: e.activation(out, in_, AF.Copy), reads=[in_], writes=[out])
        return self.op(eng, lambda e: e.tensor_copy(out, in_), reads=[in_], writes=[out])

    def memset(self, ap, val, eng='pool'):
        return self.op(eng, lambda e: e.memset(ap, val), writes=[ap])


S = 2048
D = 1024
NT = 4
EPS = 1e-6
FF = 2816
NPAIR = 22
FGROUPS = [list(range(i, min(i + 4, NPAIR))) for i in range(0, NPAIR, 4)]
WINS = [(0, 684, 0), (682, 684, 684), (1364, 684, 1366)]


def host_prep(inp):
    f = lambda a: np.ascontiguousarray(a, dtype=np.float32)
    o = {}
    def rows_tiled(w):
        K, N = w.shape
        return f(w.reshape(K // 128, 128, N).transpose(1, 0, 2))
    gains = np.stack([inp['norm_mix'][0], inp['norm_mix'][1], inp['norm_ffn'][0], inp['norm_ffn'][1],
                      inp['kv_norm'], inp['norm_final']], 0)
    o['gains'] = f(gains.reshape(6, 8, 128).transpose(2, 0, 1))
    for l in range(2):
        wu = inp['ffn_w_up'][l]
        pairs = []
        for j in range(NPAIR):
            pairs.append(np.concatenate([wu[:, j * 128:(j + 1) * 128], wu[:, FF + j * 128:FF + (j + 1) * 128]], 1))
        o[f'wup{l}'] = f(np.stack([rows_tiled(p) for p in pairs], 0))
        cw = inp['ffn_conv_w'][l]; cb = inp['ffn_conv_b'][l]
        par = np.stack([cw[0], cw[1], cw[2], cb], -1)
        par = par.reshape(2, NPAIR, 128, 4).transpose(2, 1, 0, 3)
        o[f'cpar{l}'] = f(par)
        o[f'wdn{l}'] = rows_tiled(inp['ffn_w_down'][l])
    wi = inp['gla_w_in'][0]
    hw = []
    for h in range(4):
        hw.append(np.concatenate([wi[:, h * 128:(h + 1) * 128], wi[:, 512 + h * 128:512 + (h + 1) * 128],
                                  wi[:, 1024 + h * 256:1024 + (h + 1) * 256], wi[:, 2048 + h * 256:2048 + (h + 1) * 256]], 1))
    o['gla_wh'] = f(np.stack([rows_tiled(w) for w in hw], 0))
    o['gla_wa'] = rows_tiled(wi[:, 3072:3088])
    o['gla_wau'] = f(inp['gla_w_alpha_up'][0])
    o['gla_ba'] = f(np.tile(inp['gla_b_alpha'][0][None, :], (128, 1)))
    o['gla_gn'] = f(np.tile(inp['gla_norm'][0].reshape(1, 1024), (128, 1)))
    o['gla_wo'] = rows_tiled(inp['gla_w_o'][0])
    wkv = inp['nsa_w_kv']; wq = inp['nsa_w_in'][0]
    sw = lambda w: np.concatenate([w[:, 32:64], w[:, 0:32]], 1)
    kvs, qs, gs = [], [], []
    for hk in range(4):
        c = lambda kind: wkv[:, kind * 256 + hk * 64: kind * 256 + (hk + 1) * 64]
        kvs.append(rows_tiled(np.concatenate([c(0), c(1), c(2), sw(c(2)), c(4), sw(c(4)), c(3), c(5)], 1)))
        ql = []
        for g in range(4):
            h = hk * 4 + g
            ql += [wq[:, h * 64:(h + 1) * 64], sw(wq[:, h * 64:(h + 1) * 64])]
        qs.append(rows_tiled(np.concatenate(ql, 1)))
        gcols = [1024 + b * 16 + hk * 4 + g for b in range(3) for g in range(4)]
        gs.append(rows_tiled(wq[:, gcols]))
    o['nsa_wkv'] = f(np.stack(kvs, 0)); o['nsa_wq'] = f(np.stack(qs, 0)); o['nsa_wg'] = f(np.stack(gs, 0))
    o['nsa_wo2'] = rows_tiled(inp['nsa_w_o'][0])
    o['cmp_w1'] = f(np.stack([inp['cmp_k_w1'].reshape(32, 64, 256).transpose(1, 0, 2),
                              inp['cmp_v_w1'].reshape(32, 64, 256).transpose(1, 0, 2)], 0))
    o['cmp_w2'] = f(np.stack([inp['cmp_k_w2'].reshape(2, 128, 64).transpose(1, 0, 2),
                              inp['cmp_v_w2'].reshape(2, 128, 64).transpose(1, 0, 2)], 0))
    o['cmp_peT'] = f(np.stack([inp['cmp_pe_k'].T, inp['cmp_pe_v'].T], 0))
    return o


def host_consts():
    c = {}
    c['ident'] = np.eye(128, dtype=np.float32)
    s = np.arange(128)
    c['ltri'] = (s[:, None] <= s[None, :]).astype(np.float32)
    c['utri'] = (s[:, None] > s[None, :]).astype(np.float32)
    pos = np.arange(S, dtype=np.float32)
    inv = (10000.0 ** (-np.arange(32, dtype=np.float32) / 32)).astype(np.float32)
    ang = pos[None, :] * inv[:, None]
    cos = np.cos(ang).astype(np.float32); sin = np.sin(ang).astype(np.float32)
    c['ropec'] = np.concatenate([cos, cos], 0)
    c['ropes'] = np.concatenate([-sin, sin], 0)
    n = np.arange(127)
    c['cmpmask'] = ((n[:, None] * 16 + 31) <= np.arange(S)[None, :]).astype(np.float32)
    c0 = n[:, None] * 16; s0 = np.arange(32)[None, :] * 64
    ov = np.clip(np.minimum(c0 + 32, s0 + 64) - np.maximum(c0, s0), 0, None)
    c['wov'] = (ov / 32).astype(np.float32)
    blk = np.arange(32)[None, :]; cur = (np.arange(S) // 64)[:, None]
    forced = (blk == 0) | (blk == cur) | (blk == cur - 1)
    A = np.where((blk > cur) | forced, 0.0, 1.0)
    B = np.where(blk > cur, -1.0, np.where(forced, 1e4, 0.0))
    c['selA'] = A.reshape(16, 128, 32).transpose(1, 0, 2).astype(np.float32).copy()
    c['selB'] = B.reshape(16, 128, 32).transpose(1, 0, 2).astype(np.float32).copy()
    c['expand'] = (np.arange(32)[:, None] == (np.arange(S) // 64)[None, :]).astype(np.float32)
    kp = np.arange(128)[:, None, None]; rel = np.arange(8)[None, :, None]; qq = np.arange(512)[None, None, :]
    dist = qq - (rel * 128 - 512 + kp)
    c['band'] = ((dist >= 0) & (dist < 512)).astype(np.float32)
    return {k: np.ascontiguousarray(v, dtype=np.float32) for k, v in c.items()}


class K:
    def __init__(self, nc, P, stack, nseq, shapes, stop=None):
        self.nc, self.P, self.stack, self.nseq, self.stop = nc, P, stack, nseq, stop
        self.din = {}
        for name, shp in shapes.items():
            self.din[name] = nc.dram_tensor(name, list(shp), F32, kind="ExternalInput").ap()
        self.out = nc.dram_tensor("out", [nseq, S, D], F32, kind="ExternalOutput").ap()
        self.scopes = []

    def sb(self, name, shape, dt, scoped=True):
        st = self.scopes[-1] if (scoped and self.scopes) else self.stack
        self.uid = getattr(self, 'uid', 0) + 1
        return st.enter_context(self.nc.sbuf_tensor(f"sb{self.uid}_{name}", shape, dt))

    def ps(self, name, shape, dt):
        return self.stack.enter_context(self.nc.psum_tensor("ps_" + name, shape, dt))

    def push(self):
        from contextlib import ExitStack
        es = ExitStack()
        self.scopes.append(es)

    def pop(self):
        self.P.barrier()
        self.scopes.pop().close()

    def setup(self):
        P, d = self.P, self.din
        self.xT = self.sb("xT", [128, 8, S], F32, scoped=False)
        self.rstd = self.sb("rstd", [128, S], F32, scoped=False)
        self.identf = self.sb("identf", [128, 128], F32, scoped=False)
        self.identb = self.sb("identb", [128, 128], BF16, scoped=False)
        self.onesb = self.sb("onesb", [128, 128], BF16, scoped=False)
        self.gains = self.sb("gains", [128, 6, 8], F32, scoped=False)
        self.epsc = self.sb("epsc", [128, 1], F32, scoped=False)
        self.hntmp = self.sb("hntmp", [128, 2, 512], F32, scoped=False)
        self.big0 = self.ps("big0", [128, 1024], F32)
        self.big1 = self.ps("big1", [128, 1024], F32)
        self.s0 = self.ps("s0", [128, 512], F32)
        self.s1 = self.ps("s1", [128, 512], F32)
        self.s2 = self.ps("s2", [128, 512], F32)
        self.pst = self.ps("pst", [128, 1024], BF16)
        P.dma(self.identf[:], d['ident'])
        P.dma(self.identb[:], d['ident'], eng='pool')
        P.dma(self.gains[:], d['gains'])
        P.memset(self.onesb[:], 1.0)
        P.memset(self.epsc[:], EPS)

    def load_x(self, si):
        P = self.P
        self.push()
        xin = [self.sb(f"xin{i}", [128, D], F32) for i in range(2)]
        banks = [self.s0, self.s1]
        for i in range(16):
            xb = xin[i % 2]
            P.dma(xb[:], self.din['x'][si, i * 128:(i + 1) * 128, :])
            for half in range(2):
                pb = banks[half]
                for cc in range(4):
                    c = half * 4 + cc
                    P.transpose(pb[:, cc * 128:(cc + 1) * 128], xb[:, c * 128:(c + 1) * 128], self.identf[:])
                P.copy(self.xT[:, half * 4:(half + 1) * 4, i * 128:(i + 1) * 128],
                       pb[:].rearrange("p (c t) -> p c t", c=4), eng='dve' if half == 0 else 'act')
        self.pop()

    def compute_rstd(self):
        P = self.P
        self.push()
        sqb = self.sb("sqb", [128, 8, 512], BF16)
        banks = [self.s0, self.s1]
        for t in range(NT):
            ts_ = slice(t * 512, (t + 1) * 512)
            P.act(sqb[:], self.xT[:, :, ts_], AF.Square)
            pb = banks[t % 2]
            for c in range(8):
                P.mm(pb[:], self.onesb[:], sqb[:, c, :], start=(c == 0), stop=(c == 7))
            P.act(self.rstd[:, ts_], pb[:], AF.Ln, bias=self.epsc[:, 0:1], scale=1.0 / D)
            P.act(self.rstd[:, ts_], self.rstd[:, ts_], AF.Exp, scale=-0.5)
        self.pop()

    def hn_tile(self, dst, gidx, t0, n=512, flip=0):
        P = self.P
        for c in range(8):
            if c % 2 == 0:
                P.stt(dst[:, c, 0:n], self.xT[:, c, t0:t0 + n], self.gains[:, gidx, c:c + 1], self.rstd[:, t0:t0 + n],
                      ALU.mult, ALU.mult)
            else:
                tmp = self.hntmp[:, (c // 2) % 2, 0:n]
                P.act(tmp, self.xT[:, c, t0:t0 + n], AF.Copy, scale=self.gains[:, gidx, c:c + 1])
                P.tt(dst[:, c, 0:n], tmp, self.rstd[:, t0:t0 + n], ALU.mult, eng='pool')

    def ffn(self, l):
        P, d = self.P, self.din
        self.compute_rstd()
        self.push()
        hnT = self.sb("f_hnT", [128, 8, S], BF16)
        actT = self.sb("f_actT", [128, 4, S], BF16)
        actT2 = self.sb("f_actT2", [128, 4, S], BF16)
        wu = [self.sb(f"f_wu{i}", [128, 8, 256], BF16) for i in range(3)]
        wd = [self.sb(f"f_wd{i}", [128, 4, 1024], BF16) for i in range(2)]
        cpar = self.sb("f_cpar", [128, NPAIR, 2, 4], F32)
        tg = [self.sb(f"f_tg{i}", [128, 684], F32) for i in range(2)]
        tv = [self.sb(f"f_tv{i}", [128, 684], F32) for i in range(2)]
        P.dma(cpar[:], d[f'cpar{l}'])
        for t in range(NT):
            self.hn_tile(hnT[:, :, t * 512:(t + 1) * 512], 2 + l, t * 512)
        dbanks = [self.s0, self.s1, self.s2]
        dcount = [0]
        itc = [0]
        actTs = [actT, actT2]

        def up(gi):
            grp = FGROUPS[gi]
            aT = actTs[gi % 2]
            for jj, j in enumerate(grp):
                wub = wu[j % 3]
                if j == 0:
                    P.dma(wu[0][:], d[f'wup{l}'][0], eng='pool')
                    P.dma(wu[1][:], d[f'wup{l}'][1], eng='pool')
                if j + 2 < NPAIR:
                    P.dma(wu[(j + 2) % 3][:], d[f'wup{l}'][j + 2], eng='pool')
                for (s0_, ncol, olo) in WINS:
                    tgb, tvb = tg[itc[0] % 2], tv[itc[0] % 2]
                    itc[0] += 1
                    for (pb, tb, half, silu) in ((self.big0, tgb, 0, True), (self.big1, tvb, 1, False)):
                        for (c0, cn) in ((0, 512), (512, ncol - 512)):
                            for c in range(8):
                                P.mm(pb[:, c0:c0 + cn], wub[:, c, half * 128:(half + 1) * 128],
                                     hnT[:, c, s0_ + c0:s0_ + c0 + cn], start=(c == 0), stop=(c == 7))
                            yield
                        w0 = cpar[:, j, half, 0:1]; w1 = cpar[:, j, half, 1:2]; w2 = cpar[:, j, half, 2:3]; bb = cpar[:, j, half, 3:4]
                        P.act(tb[:, 0:ncol], pb[:, 0:ncol], AF.Identity, bias=bb, scale=w2)
                        P.stt(tb[:, 1:ncol], pb[:, 0:ncol - 1], w1, tb[:, 1:ncol], ALU.mult, ALU.add)
                        P.stt(tb[:, 2:ncol], pb[:, 0:ncol - 2], w0, tb[:, 2:ncol], ALU.mult, ALU.add)
                        if silu:
                            P.act(tb[:, 0:ncol], tb[:, 0:ncol], AF.Silu)
                    lo = olo - s0_
                    P.tt(aT[:, jj, olo:s0_ + ncol], tgb[:, lo:ncol], tvb[:, lo:ncol], ALU.mult, eng='pool')
                    yield

        def down(gi):
            grp = FGROUPS[gi]
            aT = actTs[gi % 2]
            wdb = wd[gi % 2]
            if gi == 0:
                P.dma(wdb[:, 0:len(grp), :], d[f'wdn{l}'][:, grp[0]:grp[0] + len(grp), :], eng='pool')
            if gi + 1 < len(FGROUPS):
                g2 = FGROUPS[gi + 1]
                P.dma(wd[(gi + 1) % 2][:, 0:len(g2), :], d[f'wdn{l}'][:, g2[0]:g2[0] + len(g2), :], eng='pool')
            for m in range(8):
                for t in range(NT):
                    pb = dbanks[dcount[0] % 3]
                    dcount[0] += 1
                    for jj in range(len(grp)):
                        P.mm(pb[:], wdb[:, jj, m * 128:(m + 1) * 128], aT[:, jj, t * 512:(t + 1) * 512],
                             start=(jj == 0), stop=(jj == len(grp) - 1))
                    xs = self.xT[:, m, t * 512:(t + 1) * 512]
                    P.tt(xs, pb[:], xs, ALU.add)
                    yield

        def interleave(gens):
            gens = [g_ for g_ in gens if g_ is not None]
            while gens:
                for g_ in list(gens):
                    try:
                        next(g_)
                    except StopIteration:
                        gens.remove(g_)

        ng = len(FGROUPS)
        interleave([up(0)])
        for gi in range(ng):
            interleave([down(gi), up(gi + 1) if gi + 1 < ng else None])
        self.pop()

    def final_store(self, si):
        P = self.P
        self.compute_rstd()
        self.push()
        yb = [self.sb(f"o_y{i}", [128, 8, 128], F32) for i in range(2)]
        ob = [self.sb(f"o_o{i}", [128, D], F32) for i in range(2)]
        pbs = [self.big0, self.big1]
        for i in range(16):
            y = yb[i % 2]; o = ob[i % 2]; pb = pbs[i % 2]
            for c in range(8):
                P.stt(y[:, c, :], self.xT[:, c, i * 128:(i + 1) * 128], self.gains[:, 5, c:c + 1],
                      self.rstd[:, i * 128:(i + 1) * 128], ALU.mult, ALU.mult)
            for c in range(8):
                P.transpose(pb[:, c * 128:(c + 1) * 128], y[:, c, :], self.identf[:])
            P.copy(o[:, 0:512], pb[:, 0:512], eng='act')
            P.copy(o[:, 512:1024], pb[:, 512:1024], eng='dve')
            P.dma(self.out[si, i * 128:(i + 1) * 128, :], o[:])
        self.pop()

def _run_seq(self, si):
    self.load_x(si)
    if self.stop == 'load':
        return self.final_store(si)
    self.ffn(0)
    self.final_store(si)
K.run_seq = _run_seq


def _gla(self):
    P, d = self.P, self.din
    self.push()
    sb = self.sb
    oT = sb("g_oT", [128, 8, S], BF16)
    self.compute_rstd()
    self.push()
    hnT = sb("g_hnT", [128, 8, S], BF16)
    wh = [sb(f"g_wh{i}", [128, 8, 768], BF16) for i in range(2)]
    wa = sb("g_wa", [128, 8, 16], BF16)
    waus = [sb(f"g_wau{i}", [16, 128], F32) for i in range(2)]
    bas = [sb(f"g_ba{i}", [128, 128], F32) for i in range(2)]
    gn = sb("g_gn", [128, 1024], F32)
    alT = sb("g_alT", [16, S], F32)
    ltri = sb("g_ltri", [128, 128], F32)
    utri = sb("g_utri", [128, 128], F32)
    onef = sb("g_onef", [128, 128], F32)
    Sf = sb("g_Sf", [128, 256], F32)
    Sb = sb("g_Sb", [128, 256], BF16)
    NB = 2
    qk = [sb(f"g_qk{i}", [128, 256], BF16) for i in range(NB)]
    kh = [sb(f"g_kh{i}", [128, 128], BF16) for i in range(NB)]
    vv = [sb(f"g_vv{i}", [128, 256], BF16) for i in range(NB)]
    sr = [sb(f"g_sr{i}", [128, 256], F32) for i in range(NB)]
    qkT = [sb(f"g_qkT{i}", [128, 256], BF16) for i in range(NB)]
    attm = [sb(f"g_attm{i}", [128, 128], BF16) for i in range(NB)]
    ss = [sb(f"g_ss{i}", [128, 1], F32) for i in range(NB)]
    og = [sb(f"g_og{i}", [128, 256], F32) for i in range(NB)]
    sq = og
    P.dma(wa[:], d['gla_wa'], eng='pool')
    P.dma(waus[0][:], d['gla_wau'][:, 0:128])
    P.dma(bas[0][:], d['gla_ba'][:, 0:128])
    P.dma(gn[:], d['gla_gn'])
    P.dma(ltri[:], d['ltri'])
    P.dma(utri[:], d['utri'])
    P.memset(onef[:], 1.0)
    for t in range(NT):
        self.hn_tile(hnT[:, :, t * 512:(t + 1) * 512], 0, t * 512)
    for t in range(NT):
        for c in range(8):
            P.mm(self.s2[0:16, :], wa[:, c, :], hnT[:, c, t * 512:(t + 1) * 512], start=(c == 0), stop=(c == 7))
        P.copy(alT[:, t * 512:(t + 1) * 512], self.s2[0:16, :])
    ogf = [sb(f"g_ogf{i}", [128, 256], F32) for i in range(NB)]

    gsets = [dict(eb=sb(f"g_eb4{i}", [128, 512], F32), enb=sb(f"g_enb4{i}", [128, 512], F32),
                  erb=sb(f"g_erb4{i}", [128, 512], F32), ebl=sb(f"g_ebl4{i}", [128, 4], F32)) for i in range(2)]
    zb4 = sb("g_zb4", [128, 512], F32)
    gg4 = zb4

    def groupprep(h, G, gs):
        t0g = G * 512
        Z = self.s2
        for cc in range(4):
            P.mm(Z[:, cc * 128:(cc + 1) * 128], alT[0:16, t0g + cc * 128:t0g + (cc + 1) * 128], waus[h % 2][0:16, :])
        yield
        for cc in range(4):
            P.tt(zb4[:, cc * 128:(cc + 1) * 128], Z[:, cc * 128:(cc + 1) * 128], bas[h % 2][:, :], ALU.add)
        yield
        P.act(zb4[:], zb4[:], AF.Exp, scale=-1.0)
        yield
        P.act(zb4[:], zb4[:], AF.Ln, bias=onef[:, 0:1])
        yield
        P.ts(gg4[:], zb4[:], -1.0 / 16.0, None, ALU.mult)
        yield
        P.mm(Z[:, :], ltri[:], gg4[:])
        yield
        P.act(gs['eb'][:], Z[:, :], AF.Exp)
        P.act(gs['enb'][:], Z[:, :], AF.Exp, scale=-1.0)
        yield
        P.mm(Z[:, :], utri[:], gg4[:])
        yield
        P.act(gs['erb'][:], Z[:, :], AF.Exp)
        yield
        for cc in range(4):
            P.mm(Z[:, cc * 128:(cc + 1) * 128], gg4[:, cc * 128:(cc + 1) * 128], onef[:])
        yield
        P.act(gs['ebl'][:], Z[:, 0:512:128], AF.Exp)
        yield

    def phaseA(h, i, b_):
        whb = wh[h % 2]
        t0 = i * 128
        gs = gsets[(h * 4 + i // 4) % 2]
        cs = slice((i % 4) * 128, (i % 4 + 1) * 128)
        A = self.s0
        Bp = self.s1[:, 0:256]
        W = self.big0
        for c in range(8):
            P.mm(A[:], hnT[:, c, t0:t0 + 128], whb[:, c, 0:512], start=(c == 0), stop=(c == 7))
        for c in range(8):
            P.mm(Bp, hnT[:, c, t0:t0 + 128], whb[:, c, 512:768], start=(c == 0), stop=(c == 7))
        yield
        P.copy(vv[b_][:], A[:, 256:512], eng='act')
        P.act(sr[b_][:], Bp, AF.Exp, scale=-1.0)
        P.stt(qk[b_][:, 0:128], A[:, 0:128], float(128 ** -0.5), gs['eb'][:, cs], ALU.mult, ALU.mult)
        P.tt(qk[b_][:, 128:256], A[:, 128:256], gs['enb'][:, cs], ALU.mult)
        yield
        P.tt(kh[b_][:], A[:, 128:256], gs['erb'][:, cs], ALU.mult)
        P.ts(sr[b_][:], sr[b_][:], 1.0, None, ALU.add)
        P.transpose(self.pst[:, 0:128], qk[b_][:, 0:128], self.identb[:])
        P.transpose(self.pst[:, 128:256], qk[b_][:, 128:256], self.identb[:])
        yield
        P.copy(qkT[b_][:], self.pst[:, 0:256])
        P.op('dve', lambda e, a=sr[b_][:]: e.reciprocal(a, a), reads=[sr[b_][:]], writes=[sr[b_][:]])
        yield
        P.mm(W[:, 0:128], qkT[b_][:, 128:256], qkT[b_][:, 0:128])
        P.tt(sr[b_][:], Bp, sr[b_][:], ALU.mult)
        yield
        P.tt(attm[b_][:], W[:, 0:128], ltri[:], ALU.mult)
        yield

    def phaseB(h, i, b_):
        t0 = i * 128
        Wo = self.big0[:, 512:768]
        Ws = self.big1[:, 0:256]
        Wt = self.big1[:, 512:768]
        P.mm(Wo, qkT[b_][:, 0:128], Sb[:], start=True, stop=False)
        P.mm(Wo, attm[b_][:], vv[b_][:], start=False, stop=True)
        P.mm(Ws, kh[b_][:], vv[b_][:])
        yield
        P.stt(Sf[:], Sf[:], gsets[(h * 4 + i // 4) % 2]['ebl'][:, (i % 4):(i % 4) + 1], Ws, ALU.mult, ALU.add)
        P.act(sq[b_][:], Wo, AF.Square)
        yield
        P.copy(Sb[:], Sf[:], eng='pool')
        P.op('dve', lambda e, o_=ss[b_][:], i_=sq[b_][:]: e.reduce_sum(o_, i_, axis=AX.X), reads=[sq[b_][:]], writes=[ss[b_][:]])
        yield
        P.act(ss[b_][:], ss[b_][:], AF.Ln, bias=self.epsc[:, 0:1], scale=1.0 / 256)
        yield
        P.act(ss[b_][:], ss[b_][:], AF.Exp, scale=-0.5)
        yield
        P.stt(og[b_][:], Wo, ss[b_][:, 0:1], gn[:, h * 256:(h + 1) * 256], ALU.mult, ALU.mult)
        yield
        P.tt(ogf[b_][:], og[b_][:], sr[b_][:], ALU.mult, eng='pool')
        yield
        for j in range(2):
            P.transpose(Wt[:, j * 128:(j + 1) * 128], ogf[b_][:, j * 128:(j + 1) * 128], self.identf[:])
        yield
        P.copy(oT[:, 2 * h:2 * h + 2, t0:t0 + 128], Wt.rearrange("p (j t) -> p j t", j=2), eng='act')
        yield

    def interleave(gens):
        gens = [g_ for g_ in gens if g_ is not None]
        while gens:
            for g_ in list(gens):
                try:
                    next(g_)
                except StopIteration:
                    gens.remove(g_)

    P.dma(wh[0][:], d['gla_wh'][0], eng='pool')
    seq = [(h, i) for h in range(4) for i in range(16)]
    interleave([groupprep(0, 0, gsets[0])])
    interleave([phaseA(0, 0, 0)])
    for n_, (h, i) in enumerate(seq):
        if i == 0:
            P.memset(Sf[:], 0.0, eng='dve')
            P.memset(Sb[:], 0.0, eng='dve')
            if h + 1 < 4:
                P.dma(wh[(h + 1) % 2][:], d['gla_wh'][h + 1], eng='pool')
                P.dma(waus[(h + 1) % 2][:], d['gla_wau'][:, (h + 1) * 128:(h + 2) * 128])
                P.dma(bas[(h + 1) % 2][:], d['gla_ba'][:, (h + 1) * 128:(h + 2) * 128])
        nxt = seq[n_ + 1] if n_ + 1 < len(seq) else None
        prep = None
        if i % 4 == 0:
            gidx = h * 4 + i // 4 + 1
            if gidx < 16:
                prep = groupprep(gidx // 4, gidx % 4, gsets[gidx % 2])
        interleave([phaseB(h, i, n_ % NB), phaseA(nxt[0], nxt[1], (n_ + 1) % NB) if nxt else None, prep])
    self.pop()
    self.push()
    wo0 = sb("g_wo0", [128, 8, 512], BF16)
    wo1 = sb("g_wo1", [128, 8, 512], BF16)
    P.dma(wo0[:], d['gla_wo'][:, :, 0:512], eng='pool')
    P.dma(wo1[:], d['gla_wo'][:, :, 512:1024], eng='pool')
    banks = [self.s0, self.s1, self.s2]
    n = 0
    for m in range(8):
        wb = wo0 if m < 4 else wo1
        mm_ = m % 4
        for t in range(NT):
            pb = banks[n % 3]
            n += 1
            for c in range(8):
                P.mm(pb[:], wb[:, c, mm_ * 128:(mm_ + 1) * 128], oT[:, c, t * 512:(t + 1) * 512], start=(c == 0), stop=(c == 7))
            xs = self.xT[:, m, t * 512:(t + 1) * 512]
            P.tt(xs, pb[:], xs, ALU.add)
    self.pop()
    self.pop()


K.gla = _gla


def _run_seq(self, si):
    self.load_x(si)
    st = self.stop
    if st == 'load':
        return self.final_store(si)
    if st == 'ffn0':
        self.ffn(0)
        return self.final_store(si)
    self.gla()
    if st == 'gla':
        return self.final_store(si)
    self.final_store(si)


K.run_seq = _run_seq


BIG = 30000.0
SCALE = 0.125


def _nsa(self):
    P, d = self.P, self.din
    sb = self.sb
    self.compute_rstd()
    self.push()
    oT = sb("n_oT", [128, 8, S], BF16)
    LK = sb("n_LK", [96, S], BF16)
    kwT = sb("n_kwT", [64, S], BF16)
    V1 = sb("n_V1", [128, 16, 2, 65], BF16)
    kcT = sb("n_kcT", [64, 128], BF16)
    vc = sb("n_vc", [128, 64], BF16)
    hnb = sb("n_hnb", [128, 8, 512], BF16)
    rcsK = sb("n_rcsK", [128, 512], F32)
    r12K = [sb(f"n_r12K{i}", [128, 512], F32) for i in range(2)]
    r2sK0 = sb("n_r2sK0", [64, 512], F32)
    r2sK = [r2sK0, r2sK0]
    P.dma(LK[64:96, :], d['expand'], eng='pool')
    sc_banks = [self.s0, self.s1, self.s2]
    scn = [0]

    def scbank():
        b = sc_banks[scn[0] % 3]
        scn[0] += 1
        return b

    def rope_to(dst, pa, k):
        P.tt(r12K[k][:], pa, rcsK[:], ALU.mult)
        P.copy(r2sK[k][:], r12K[k][64:128, :])
        P.tt(dst, r12K[k][0:64, :], r2sK[k][:], ALU.add, eng='pool')

    for hk in range(4):
        self.push()
        wkv = sb("n_wkv", [128, 8, 512], BF16)
        rawT = sb("n_rawT", [64, 2, S], BF16)
        w1bs = [sb(f"n_w1b{i}", [64, 32, 256], BF16) for i in range(2)]
        w2bs = [sb(f"n_w2b{i}", [128, 2, 64], BF16) for i in range(2)]
        pefs = [sb(f"n_pef{i}", [64, 32], F32) for i in range(2)]
        hnb2 = sb("n_hnb2", [128, 8, 512], BF16)
        pe2 = sb("n_pe2", [64, 32, 2], BF16)
        cb = sb("n_cb", [128, 2], F32)
        hx = sb("n_hx", [128, 127], F32)
        hx2 = sb("n_hx2", [128, 127], F32)
        hidT = sb("n_hidT", [128, 2, 127], BF16)
        P.dma(wkv[:], d['nsa_wkv'][hk], eng='pool')
        for kind in range(2):
            P.dma(w1bs[kind][:], d['cmp_w1'][kind], eng='pool')
            P.dma(w2bs[kind][:], d['cmp_w2'][kind], eng='pool')
            P.dma(pefs[kind][:], d['cmp_peT'][kind])
        P.memset(V1[:], 1.0, eng='dve')
        hnb_q = hnb
        for tt in range(NT):
            t0 = tt * 512
            hnb = hnb_q if tt % 2 == 0 else hnb2
            self.hn_tile(hnb, 4, t0)
            P.dma(rcsK[0:64, :], d['ropec'][:, t0:t0 + 512])
            P.dma(rcsK[64:128, :], d['ropes'][:, t0:t0 + 512])
            for u in range(2):
                pb = scbank()
                for c in range(8):
                    P.mm(pb[0:64, 0:512], wkv[:, c, u * 64:(u + 1) * 64], hnb[:, c, :], start=(c == 0), stop=(c == 7))
                P.copy(rawT[:, u, t0:t0 + 512], pb[0:64, 0:512], eng='act')
            for k, (pb, dst) in enumerate(((self.big0[:, 0:512], LK[0:64, t0:t0 + 512]), (self.big1[:, 0:512], kwT[0:64, t0:t0 + 512]))):
                for c in range(8):
                    P.mm(pb[:, 0:512], wkv[:, c, 128 + k * 128:256 + k * 128], hnb[:, c, :], start=(c == 0), stop=(c == 7))
                rope_to(dst, pb[:, 0:512], k)
            for sub in range(4):
                pb = scbank()
                for c in range(8):
                    P.mm(pb[:, 0:128], hnb[:, c, sub * 128:(sub + 1) * 128], wkv[:, c, 384:512], start=(c == 0), stop=(c == 7))
                P.copy(V1[:, tt * 4 + sub, :, 0:64], pb[:, 0:128].rearrange("p (b e) -> p b e", b=2), eng='act')
        hnb = hnb_q
        for kind in range(2):
            w1b = w1bs[kind]; w2b = w2bs[kind]; pef = pefs[kind]
            P.copy(pe2[:, :, 0], pef[:])
            P.copy(pe2[:, :, 1], pef[:])
            for ht in range(2):
                pb = scbank()
                for j in range(32):
                    P.mm(pb[:, 0:2], w1b[:, j, ht * 128:(ht + 1) * 128], pe2[:, j, :], start=(j == 0), stop=(j == 31))
                P.copy(cb[:, ht:ht + 1], pb[:, 0:1])
                pb = scbank()
                for j in range(32):
                    P.mm(pb[:, 0:127], w1b[:, j, ht * 128:(ht + 1) * 128], rawT[0:64, kind, j:j + 2017:16],
                         start=(j == 0), stop=(j == 31))
                P.act(hx[:], pb[:, 0:127], AF.Identity, bias=cb[:, ht:ht + 1])
                P.tt(hx2[:], hx[:], hx[:], ALU.mult)
                P.ts(hx2[:], hx2[:], 0.044715, 1.0, ALU.mult, ALU.add)
                P.tt(hx2[:], hx2[:], hx[:], ALU.mult)
                P.act(hx2[:], hx2[:], AF.Exp, scale=-1.5957691216057308)
                P.ts(hx2[:], hx2[:], 1.0, None, ALU.add)
                P.op('dve', lambda e, a=hx2[:]: e.reciprocal(a, a), reads=[hx2[:]], writes=[hx2[:]])
                P.tt(hidT[:, ht, :], hx[:], hx2[:], ALU.mult)
            pb = scbank()
            if kind == 0:
                for ht in range(2):
                    P.mm(pb[0:64, 0:127], w2b[:, ht, :], hidT[:, ht, :], start=(ht == 0), stop=(ht == 1))
                P.copy(kcT[:, 0:127], pb[0:64, 0:127])
            else:
                for ht in range(2):
                    P.mm(pb[0:127, 0:64], hidT[:, ht, :], w2b[:, ht, :], start=(ht == 0), stop=(ht == 1))
                P.copy(vc[0:127, :], pb[0:127, 0:64])
        self.pop()
        self.push()
        wq = sb("n_wq", [128, 8, 512], BF16)
        wg = sb("n_wg", [128, 8, 12], BF16)
        RQs = [sb(f"n_RQ{i}", [96, 4, 512], BF16) for i in range(2)]
        qnat = sb("n_qnat", [64, 4, 512], BF16)
        band = sb("n_band", [128, 8, 512], BF16)
        cmask = sb("n_cmask", [128, 512], BF16)
        selA = sb("n_selA", [128, 4, 32], F32)
        selB = sb("n_selB", [128, 4, 32], F32)
        wov = sb("n_wov", [128, 32], BF16)
        NPT = 8
        pT = [sb(f"n_pT{i}", [128, 512], BF16) for i in range(NPT)]
        cpt = [sb(f"n_cpt{i}", [128, 512], BF16) for i in range(2)]
        dens = [sb(f"n_den{i}", [128, 512], F32) for i in range(2)]
        pns = [sb(f"n_pn{i}", [128, 512], BF16) for i in range(2)]
        gts = [sb(f"n_gt{i}", [128, 4, 12], F32) for i in range(2)]
        otoks = [sb(f"n_otok{i}", [128, 4, 256], F32) for i in range(2)]
        otb = sb("n_otb", [128, 4, 256], BF16)
        impa = sb("n_impa", [128, 4, 32], F32)
        imp2 = sb("n_imp2", [128, 32], F32)
        imp3 = sb("n_imp3", [128, 32], F32)
        m8 = sb("n_m8", [128, 16], F32)
        msel = sb("n_msel", [128, 32], F32)
        negm = sb("n_negm", [128, 4, 96], BF16)
        rden = sb("n_rden", [128, 4], F32)
        fac = sb("n_fac", [128, 4], F32)
        rcs = sb("n_rcs", [128, 512], F32)
        r12s = [sb(f"n_r12{i}", [128, 512], F32) for i in range(2)]
        r2ss = [sb(f"n_r2s{i}", [64, 512], F32) for i in range(2)]
        P.dma(wq[:], d['nsa_wq'][hk], eng='pool')
        P.dma(wg[:], d['nsa_wg'][hk], eng='pool')
        P.dma(band[:], d['band'], eng='pool')
        P.dma(wov[0:127, :], d['wov'], eng='pool')
        P.memset(negm[:], 0.0, eng='dve')
        pti = [0]

        def next_pT():
            b = pT[pti[0] % NPT]
            pti[0] += 1
            return b

        B1 = [self.big1[:, 0:512], self.big1[:, 512:1024]]

        def prologue(qt):
            q0 = qt * 512
            qb = q0 // 128
            RQ = RQs[qt % 2]; gt = gts[qt % 2]; otok = otoks[qt % 2]
            self.hn_tile(hnb, 1, q0)
            P.dma(rcs[0:64, :], d['ropec'][:, q0:q0 + 512])
            P.dma(rcs[64:128, :], d['ropes'][:, q0:q0 + 512])
            P.dma(cmask[0:127, :], d['cmpmask'][:, q0:q0 + 512], eng='pool')
            P.dma(selA[:], d['selA'][:, qb:qb + 4, :])
            P.dma(selB[:], d['selB'][:, qb:qb + 4, :])
            yield
            for sub in range(4):
                pb = B1[sub % 2]
                for c in range(8):
                    P.mm(pb[:, 0:12], hnb[:, c, sub * 128:(sub + 1) * 128], wg[:, c, :], start=(c == 0), stop=(c == 7))
                P.act(gt[:, sub, :], pb[:, 0:12], AF.Exp, scale=-1.0)
                yield
            P.ts(gt[:], gt[:], 1.0, None, ALU.add)
            P.op('dve', lambda e, a=gt[:]: e.reciprocal(a, a), reads=[gt[:]], writes=[gt[:]])
            P.memset(impa[:], 0.0, eng='dve')

            def gchain(g):
                pa = B1[g % 2]
                r12 = r12s[g % 2]; r2s = r2ss[g % 2]
                for c in range(8):
                    P.mm(pa[:, :], wq[:, c, g * 128:(g + 1) * 128], hnb[:, c, :], start=(c == 0), stop=(c == 7))
                yield
                P.copy(qnat[:, g, :], pa[0:64, :], eng='act')
                P.tt(r12[:], pa[:, :], rcs[:], ALU.mult)
                yield
                P.copy(r2s[:], r12[64:128, :], eng='pool')
                yield
                P.tt(RQ[0:64, g, :], r12[0:64, :], r2s[:], ALU.add, eng='pool')
                yield
                pb = B1[g % 2]
                pt = cpt[g % 2]; den = dens[g % 2]; pn = pns[g % 2]
                P.mm(pb[0:127, :], kcT[:, 0:127], qnat[:, g, :])
                yield
                P.act(pt[0:127, :], pb[0:127, :], AF.Exp, scale=SCALE)
                yield
                P.tt(pt[0:127, :], pt[0:127, :], cmask[0:127, :], ALU.mult)
                yield
                P.mm(pb[:, :], self.onesb[0:127, :], pt[0:127, :])
                yield
                P.ts(den[:], pb[:, :], 1e-30, None, ALU.add)
                yield
                P.op('dve', lambda e, a=den[:]: e.reciprocal(a, a), reads=[den[:]], writes=[den[:]])
                yield
                P.tt(pn[0:127, :], pt[0:127, :], den[0:127, :], ALU.mult)
                yield
                for sub in range(4):
                    P.mm(pb[:, sub * 96:sub * 96 + 64], pn[0:127, sub * 128:(sub + 1) * 128], vc[0:127, :])
                    P.mm(pb[:, sub * 96 + 64:sub * 96 + 96], pn[0:127, sub * 128:(sub + 1) * 128], wov[0:127, :])
                yield
                pv = pb[:, 0:384].rearrange("p (s c) -> p s c", c=96)
                for sub in range(4):
                    P.ts(otok[:, sub, g * 64:(g + 1) * 64], pv[:, sub, 0:64], gt[:, sub, g:g + 1], None, ALU.mult)
                P.tt(impa[:], pv[:, :, 64:96], impa[:], ALU.add)
                yield

            for gp in range(2):
                ga, gb = gchain(2 * gp), gchain(2 * gp + 1)
                alive = [ga, gb]
                while alive:
                    for g_ in list(alive):
                        try:
                            next(g_)
                        except StopIteration:
                            alive.remove(g_)
                    yield
            for sub in range(4):
                P.tt(imp2[:], impa[:, sub, :], selA[:, sub, :], ALU.mult)
                P.tt(imp2[:], imp2[:], selB[:, sub, :], ALU.add)
                yield
                P.op('dve', lambda e: e.max(m8[:, 0:8], imp2[:]), reads=[imp2[:]], writes=[m8[:, 0:8]])
                P.op('dve', lambda e: e.match_replace(imp3[:], m8[:, 0:8], imp2[:], -1e30), reads=[m8[:, 0:8], imp2[:]], writes=[imp3[:]])
                yield
                P.op('dve', lambda e: e.max(m8[:, 8:16], imp3[:]), reads=[imp3[:]], writes=[m8[:, 8:16]])
                P.ts(msel[:], imp2[:], m8[:, 15:16], None, ALU.is_ge)
                yield
                P.ts(negm[:, sub, 64:96], msel[:], 1.0, BIG, ALU.subtract, ALU.mult)
                yield
                P.transpose(self.pst[0:96, 512 + sub * 128:512 + (sub + 1) * 128], negm[:, sub, :], self.identb[:])
                yield
            for g in range(4):
                P.copy(RQ[64:96, g, :], self.pst[64:96, 512:1024], eng='dve')
            yield

        def mainloop(qt):
            q0 = qt * 512
            qb = q0 // 128
            RQ = RQs[qt % 2]; gt = gts[qt % 2]; otok = otoks[qt % 2]
            tiles = []
            for br in range(2):
                for g in range(4):
                    kts = list(range(0, qb + 4)) if br == 0 else list(range(max(0, qb - 4), qb + 4))
                    for ii, kt in enumerate(kts):
                        tiles.append((br, g, kt, ii == 0, ii == len(kts) - 1))
            LOOK = 4
            pts = {}
            main_banks = [self.s0, self.s1, self.s2]

            def st_score(i):
                br, g, kt, first, last = tiles[i]
                r = kt - qb + 4
                pb = main_banks[i % len(main_banks)]
                masked = (br == 1 or r >= 4)
                if br == 0:
                    P.mm(pb[:, 0:512], LK[0:96, kt * 128:(kt + 1) * 128], RQ[0:96, g, :])
                else:
                    P.mm(pb[:, 0:512], kwT[0:64, kt * 128:(kt + 1) * 128], RQ[0:64, g, :])
                pt = next_pT()
                pts[i] = pt
                P.act(pt[:], pb[:, 0:512], AF.Exp, scale=SCALE)
                if masked:
                    P.tt(pt[:], pt[:], band[:, r, :], ALU.mult)

            def st_pv(i):
                br, g, kt, first, last = tiles[i]
                r = kt - qb + 4
                O = self.big0[:, 0:260] if (g % 2 == 0) else self.big0[:, 512:772]
                pt = pts.pop(i)
                if first:
                    P.memset(O, 0.0, eng='dve')
                for sub in range(4):
                    if r - sub > 4 or (br == 1 and r - sub < 0):
                        continue
                    P.mm(O[:, sub * 65:(sub + 1) * 65], pt[:, sub * 128:(sub + 1) * 128], V1[:, kt, br, :],
                         start=False, stop=False, skip=True)
                if last:
                    Ov = O.rearrange("p (s c) -> p s c", c=65)
                    P.op('dve', lambda e, o_=rden[:], i_=Ov[:, :, 64]: e.reciprocal(o_, i_), reads=[O], writes=[rden[:]])
                    P.tt(fac[:], rden[:], gt[:, :, (br + 1) * 4 + g], ALU.mult)
                    for sub in range(4):
                        os_ = otok[:, sub, g * 64:(g + 1) * 64]
                        P.stt(os_, Ov[:, sub, 0:64], fac[:, sub:sub + 1], os_, ALU.mult, ALU.add)

            for i in range(len(tiles) + LOOK):
                if i < len(tiles):
                    st_score(i)
                if i - LOOK >= 0:
                    st_pv(i - LOOK)
                yield
            P.copy(otb[:], otok[:], eng='pool')
            for sub in range(4):
                for j in range(2):
                    P.transpose(self.pst[:, j * 128:(j + 1) * 128], otb[:, sub, j * 128:(j + 1) * 128], self.identb[:])
                P.copy(oT[:, 2 * hk:2 * hk + 2, q0 + sub * 128:q0 + (sub + 1) * 128],
                       self.pst[:, 0:256].rearrange("p (j t) -> p j t", j=2), eng='act')
            yield

        def interleave(gens):
            gens = [g_ for g_ in gens if g_ is not None]
            while gens:
                for g_ in list(gens):
                    try:
                        next(g_)
                    except StopIteration:
                        gens.remove(g_)

        order = [3, 2, 1, 0]
        interleave([prologue(order[0])])
        for oi, qt in enumerate(order):
            interleave([mainloop(qt), prologue(order[oi + 1]) if oi + 1 < NT else None])
        self.pop()
    self.push()
    wo0 = sb("n_wo0", [128, 8, 512], BF16)
    wo1 = sb("n_wo1", [128, 8, 512], BF16)
    P.dma(wo0[:], d['nsa_wo2'][:, :, 0:512], eng='pool')
    P.dma(wo1[:], d['nsa_wo2'][:, :, 512:1024], eng='pool')
    n = 0
    for m in range(8):
        wb = wo0 if m < 4 else wo1
        mm_ = m % 4
        for t in range(NT):
            pb = sc_banks[n % 3]
            n += 1
            for c in range(8):
                P.mm(pb[:], wb[:, c, mm_ * 128:(mm_ + 1) * 128], oT[:, c, t * 512:(t + 1) * 512], start=(c == 0), stop=(c == 7))
            xs = self.xT[:, m, t * 512:(t + 1) * 512]
            P.tt(xs, pb[:], xs, ALU.add)
    self.pop()
    self.pop()


K.nsa = _nsa


def _run_seq(self, si):
    self.load_x(si)
    st = self.stop
    if st == 'load':
        return self.final_store(si)
    if st == 'ffn0':
        self.ffn(0)
        return self.final_store(si)
    if st == 'nsa':
        self.nsa()
        return self.final_store(si)
    self.gla()
    if st == 'gla':
        return self.final_store(si)
    self.ffn(0)
    self.nsa()
    self.ffn(1)
    self.final_store(si)


K.run_seq = _run_seq


N_CORES = 8


def build_program(nseq, shapes, stop=None):
    from contextlib import ExitStack
    nc = bass.Bass("TRN2", target_bir_lowering=False)
    with ExitStack() as stack:
        P = Prog(nc)
        k = K(nc, P, stack, nseq, shapes, stop)
        k.setup()
        for si in range(nseq):
            k.run_seq(si)
        P.emit(stack)
    return nc, P


def run(inputs, n_cores=N_CORES, stop=None, trace=False):
    x = np.ascontiguousarray(inputs['x'], dtype=np.float32)
    B = x.shape[0]
    nseq = B // n_cores
    shared = host_prep(inputs)
    shared.update(host_consts())
    shapes = {k: v.shape for k, v in shared.items()}
    shapes['x'] = (nseq, S, D)
    nc, P = build_program(nseq, shapes, stop)
    in_maps = []
    for c in range(n_cores):
        m = dict(shared)
        m['x'] = x[c * nseq:(c + 1) * nseq]
        in_maps.append(m)
    res = run_bass_kernel_spmd(nc, in_maps, core_ids=list(range(n_cores)), trace=trace)
    out = np.concatenate([r['out'] for r in res.results], axis=0)
    return out, res, P


def kernel(**inputs):
    out, _, _ = run(inputs)
    return out.astype(np.float32)
```
